# Optimizing a Trainium2 kernel written in Bass

```python
import math
import jax
import jax.numpy as jnp
from jax import lax
import numpy as np

D_MODEL = 2048
BATCH = 8
SEQ = 2048
DEPTH = 4

GRID_W = 64
CTX_LEN = 256
HEAD_DIM = 128
ROPE_THETA = 10000.0
LN_EPS = 1e-5
NEG_INF = -1e30
N_MIXERS = 4
DEEPNORM_ALPHA = (2.0 * DEPTH) ** 0.25
DEEPNORM_BETA = (8.0 * DEPTH) ** -0.25

NA_HEADS = D_MODEL // HEAD_DIM
NA_KH = 8
NA_KW = 16
CONV_WIDTH = 31
SWA_HEADS = D_MODEL // HEAD_DIM
SWA_KV_HEADS = SWA_HEADS // 4
SWA_WINDOW = 128
SWA_BLOCK = 128
DIFF_HEADS = D_MODEL // (2 * HEAD_DIM)
DIFF_BLOCK = 128
MOE_GROUPS = 4
MOE_EXPERTS_PER_GROUP = 8
MOE_EXPERTS = MOE_GROUPS * MOE_EXPERTS_PER_GROUP
MOE_TOP_K = 2
MOE_D_FF = D_MODEL // 4
MOE_BLOCK = 256

kernel_name = 'hybrid_dit_interleaved_hmoe'


def layer_norm(x, g, b):
    xf = x.astype(jnp.float32)
    mu = jnp.mean(xf, -1, keepdims=True)
    var = jnp.mean(jnp.square(xf - mu), -1, keepdims=True)
    return ((xf - mu) * lax.rsqrt(var + LN_EPS)).astype(x.dtype) * g + b


def rms_norm(x, g):
    xf = x.astype(jnp.float32)
    return (xf * lax.rsqrt(jnp.mean(jnp.square(xf), -1, keepdims=True) + LN_EPS)).astype(x.dtype) * g


def softmax_f32(s):
    return jax.nn.softmax(s.astype(jnp.float32), axis=-1)


def axial_rope_tables(n_tokens):
    t = jnp.arange(n_tokens, dtype=jnp.int32)
    pos = jnp.stack([t // GRID_W, t % GRID_W], -1).astype(jnp.float32)
    n_freq = HEAD_DIM // 4
    inv_freq = ROPE_THETA ** (-jnp.arange(n_freq, dtype=jnp.float32) / n_freq)
    ang = pos[:, :, None] * inv_freq
    return jnp.cos(ang), jnp.sin(ang)


def apply_axial_rope(x, cos, sin):
    shp = x.shape
    xr = x.reshape(shp[:-1] + (2, 2, HEAD_DIM // 4))
    x1, x2 = xr[..., 0, :], xr[..., 1, :]
    cs, sn = cos.astype(x.dtype), sin.astype(x.dtype)
    out = jnp.stack([x1 * cs - x2 * sn, x2 * cs + x1 * sn], axis=-2)
    return out.reshape(shp)


def adaln_modulation(cvec, w, b):
    m = jax.nn.silu(cvec) @ w + b
    return jnp.split(m, 6, axis=-1)


def neighbourhood_attention(u, uc, w_qkv, rpb, w_o):
    B, L, _ = u.shape
    C = uc.shape[1]
    H, Dh = NA_HEADS, HEAD_DIM
    rows = L // GRID_W
    kh = min(NA_KH, rows)
    scale = Dh ** -0.5

    def heads(h):
        n = h.shape[1]
        qkv = (h @ w_qkv).reshape(B, n, 3, H, Dh)
        return jnp.transpose(qkv, (2, 0, 3, 1, 4))

    q, k, v = heads(u)
    qc, kc, vc = heads(uc)
    pc = softmax_f32(jnp.einsum('bhqd,bhkd->bhqk', qc, kc) * scale).astype(vc.dtype)
    oc = jnp.einsum('bhqk,bhkd->bhqd', pc, vc)
    qg = q.reshape(B, H, rows, GRID_W, Dh)
    kg = k.reshape(B, H, rows, GRID_W, Dh)
    vg = v.reshape(B, H, rows, GRID_W, Dh)
    cols = jnp.arange(GRID_W)
    col_start = jnp.clip(cols - NA_KW // 2, 0, GRID_W - NA_KW)
    col_ok = (cols[None, :] >= col_start[:, None]) & (cols[None, :] < col_start[:, None] + NA_KW)
    dc_idx = jnp.clip(cols[None, :] - cols[:, None] + NA_KW - 1, 0, 2 * NA_KW - 2)
    rpb_cols = jnp.take(rpb, dc_idx, axis=2)

    def row_block(r):
        r0 = jnp.clip(r - kh // 2, 0, rows - kh)
        k_blk = lax.dynamic_slice_in_dim(kg, r0, kh, axis=2)
        v_blk = lax.dynamic_slice_in_dim(vg, r0, kh, axis=2)
        q_row = lax.dynamic_index_in_dim(qg, r, axis=2, keepdims=False)
        dr_idx = r0 + jnp.arange(kh) - r + NA_KH - 1
        bias = jnp.transpose(jnp.take(rpb_cols, dr_idx, axis=1), (0, 2, 1, 3))
        s_loc = jnp.einsum('bhqd,bhikd->bhqik', q_row, k_blk).astype(jnp.float32) * scale + bias.astype(jnp.float32)
        s_loc = jnp.where(col_ok[:, None, :], s_loc, NEG_INF).reshape(B, H, GRID_W, kh * GRID_W)
        s_ctx = jnp.einsum('bhqd,bhkd->bhqk', q_row, kc).astype(jnp.float32) * scale
        p = softmax_f32(jnp.concatenate([s_loc, s_ctx], -1)).astype(v.dtype)
        p_loc = p[..., :kh * GRID_W].reshape(B, H, GRID_W, kh, GRID_W)
        return (jnp.einsum('bhqik,bhikd->bhqd', p_loc, v_blk)
                + jnp.einsum('bhqk,bhkd->bhqd', p[..., kh * GRID_W:], vc))

    o = lax.map(row_block, jnp.arange(rows))
    o = jnp.transpose(o, (1, 0, 3, 2, 4)).reshape(B, L, H * Dh)
    oc = jnp.transpose(oc, (0, 2, 1, 3)).reshape(B, C, H * Dh)
    return o @ w_o, oc @ w_o


def conformer_conv(u, uc, w_in, b_in, dw, dw_b, ln_g, ln_b, w_out, b_out):
    D = u.shape[-1]
    pad = CONV_WIDTH // 2

    def run(h):
        a = h @ w_in + b_in
        h = a[..., :D] * jax.nn.sigmoid(a[..., D:])
        h = lax.conv_general_dilated(h, dw[:, None, :], window_strides=(1,), padding=[(pad, pad)],
                                     dimension_numbers=('NWC', 'WIO', 'NWC'),
                                     feature_group_count=D) + dw_b
        h = jax.nn.silu(layer_norm(h, ln_g, ln_b))
        return h @ w_out + b_out

    return run(u), run(uc)


def window_sink_attention(u, uc, w_qkv, sink, w_o, cos, sin):
    B, L, D = u.shape
    C = uc.shape[1]
    G, R, Dh = SWA_KV_HEADS, SWA_HEADS // SWA_KV_HEADS, HEAD_DIM
    nb = L // SWA_BLOCK
    scale = Dh ** -0.5
    nq = G * R * Dh

    def heads(h):
        n = h.shape[1]
        a = h @ w_qkv
        q = a[..., :nq].reshape(B, n, G, R, Dh).transpose(0, 2, 3, 1, 4)
        k = a[..., nq:nq + G * Dh].reshape(B, n, G, Dh).transpose(0, 2, 1, 3)
        v = a[..., nq + G * Dh:].reshape(B, n, G, Dh).transpose(0, 2, 1, 3)
        return q, k, v

    q, k, v = heads(u)
    q, k = apply_axial_rope(q, cos, sin), apply_axial_rope(k, cos, sin)
    qc, kc, vc = heads(uc)
    sink_f = sink.astype(jnp.float32).reshape(1, G, R, 1, 1)
    s_c = jnp.einsum('bgrqd,bgkd->bgrqk', qc, kc).astype(jnp.float32) * scale
    s_c = jnp.concatenate([s_c, jnp.broadcast_to(sink_f, (B, G, R, C, 1))], -1)
    pc = softmax_f32(s_c)[..., :C].astype(vc.dtype)
    oc = jnp.einsum('bgrqk,bgkd->bgrqd', pc, vc)
    pad = SWA_WINDOW
    span = SWA_BLOCK + 2 * pad
    kp = jnp.pad(k, ((0, 0), (0, 0), (pad, pad), (0, 0)))
    vp = jnp.pad(v, ((0, 0), (0, 0), (pad, pad), (0, 0)))
    qb = jnp.moveaxis(q.reshape(B, G, R, nb, SWA_BLOCK, Dh), 3, 0)
    sink_q = jnp.broadcast_to(sink_f, (B, G, R, SWA_BLOCK, 1))
    qi = jnp.arange(SWA_BLOCK)[:, None]
    kj = jnp.arange(span)[None, :]

    def block(args):
        n, q_blk = args
        start = n * SWA_BLOCK
        k_blk = lax.dynamic_slice_in_dim(kp, start, span, axis=2)
        v_blk = lax.dynamic_slice_in_dim(vp, start, span, axis=2)
        kpos = start - pad + kj
        qpos = start + qi
        ok = (jnp.abs(qpos - kpos) <= SWA_WINDOW) & (kpos >= 0) & (kpos < L)
        s_loc = jnp.where(ok, jnp.einsum('bgrqd,bgkd->bgrqk', q_blk, k_blk).astype(jnp.float32) * scale, NEG_INF)
        s_ctx = jnp.einsum('bgrqd,bgkd->bgrqk', q_blk, kc).astype(jnp.float32) * scale
        p = softmax_f32(jnp.concatenate([s_loc, s_ctx, sink_q], -1)).astype(v.dtype)
        return (jnp.einsum('bgrqk,bgkd->bgrqd', p[..., :span], v_blk)
                + jnp.einsum('bgrqk,bgkd->bgrqd', p[..., span:span + C], vc))

    o = lax.map(block, (jnp.arange(nb), qb))
    o = jnp.transpose(o, (1, 0, 4, 2, 3, 5)).reshape(B, L, nq)
    oc = jnp.transpose(oc, (0, 3, 1, 2, 4)).reshape(B, C, nq)
    return o @ w_o, oc @ w_o


def differential_attention(u, uc, w_qkv, lam, subln_g, w_o, cos, sin, lambda_init):
    B, L, D = u.shape
    C = uc.shape[1]
    H, Dh = DIFF_HEADS, HEAD_DIM
    nb = L // DIFF_BLOCK
    scale = Dh ** -0.5
    lam_f = lam.astype(jnp.float32)
    lmbda = jnp.exp(jnp.sum(lam_f[0] * lam_f[1])) - jnp.exp(jnp.sum(lam_f[2] * lam_f[3])) + lambda_init

    def heads(h):
        n = h.shape[1]
        a = h @ w_qkv
        q = a[..., :2 * H * Dh].reshape(B, n, H, 2, Dh).transpose(0, 2, 3, 1, 4)
        k = a[..., 2 * H * Dh:4 * H * Dh].reshape(B, n, H, 2, Dh).transpose(0, 2, 3, 1, 4)
        v = a[..., 4 * H * Dh:].reshape(B, n, H, 2 * Dh).transpose(0, 2, 1, 3)
        return q, k, v

    def diff_attend(qq, kk, vv):
        p = softmax_f32(jnp.einsum('bhmqd,bhmkd->bhmqk', qq, kk) * scale)
        pd = (p[:, :, 0] - lmbda * p[:, :, 1]).astype(vv.dtype)
        return jnp.einsum('bhqk,bhkd->bhqd', pd, vv)

    def finish(o):
        o = rms_norm(o, subln_g) * (1.0 - lambda_init)
        n = o.shape[2]
        return o.transpose(0, 2, 1, 3).reshape(B, n, H * 2 * Dh) @ w_o

    q, k, v = heads(u)
    q, k = apply_axial_rope(q, cos, sin), apply_axial_rope(k, cos, sin)
    qc, kc, vc = heads(uc)
    oc = diff_attend(qc, kc, vc)
    k_all = jnp.concatenate([k, kc], axis=3)
    v_all = jnp.concatenate([v, vc], axis=2)
    qb = jnp.moveaxis(q.reshape(B, H, 2, nb, DIFF_BLOCK, Dh), 3, 0)
    o = lax.map(lambda qq: diff_attend(qq, k_all, v_all), qb)
    o = jnp.moveaxis(o, 0, 2).reshape(B, H, L, 2 * Dh)
    return finish(o), finish(oc)


def grouped_expert_ffn(h, expert, gate, w13, w2):
    N, D = h.shape
    A = N * MOE_TOP_K
    flat_e = expert.reshape(A)
    order = jnp.argsort(flat_e)
    e_sorted = flat_e[order]
    counts = jnp.bincount(flat_e, length=MOE_EXPERTS)
    padded = (counts + MOE_BLOCK - 1) // MOE_BLOCK * MOE_BLOCK
    seg_start = jnp.cumsum(counts) - counts
    pad_end = jnp.cumsum(padded)
    pad_start = pad_end - padded
    slot = pad_start[e_sorted] + (jnp.arange(A) - seg_start[e_sorted])
    n_blocks = -(-A // MOE_BLOCK) + MOE_EXPERTS
    n_slots = n_blocks * MOE_BLOCK
    tok = jnp.full((n_slots,), N, dtype=jnp.int32).at[slot].set((order // MOE_TOP_K).astype(jnp.int32))
    g_slot = jnp.zeros((n_slots,), gate.dtype).at[slot].set(gate.reshape(A)[order])
    blk_expert = jnp.minimum(jnp.searchsorted(pad_end, jnp.arange(n_blocks) * MOE_BLOCK, side='right'),
                             MOE_EXPERTS - 1)
    h_pad = jnp.concatenate([h, jnp.zeros((1, D), h.dtype)], 0)
    xs = h_pad[tok].reshape(n_blocks, MOE_BLOCK, D)

    def expert_block(args):
        xb, e = args
        a = xb @ w13[e]
        return (jax.nn.silu(a[:, :MOE_D_FF]) * a[:, MOE_D_FF:]) @ w2[e]

    ys = lax.map(expert_block, (xs, blk_expert)).reshape(n_slots, D)
    out = jnp.zeros((N + 1, D), h.dtype).at[tok].add(ys * g_slot[:, None])
    return out[:N]


def hierarchical_moe(h, rg_w, rg_b, re_w, re_b, w13, w2):
    N = h.shape[0]
    g_prob = softmax_f32(h @ rg_w + rg_b)
    g_p, g_idx = lax.top_k(g_prob, 1)
    e_logits = (h @ re_w + re_b).astype(jnp.float32).reshape(N, MOE_GROUPS, MOE_EXPERTS_PER_GROUP)
    e_logits = e_logits[jnp.arange(N), g_idx[:, 0]]
    e_p, e_idx = lax.top_k(jax.nn.softmax(e_logits, axis=-1), MOE_TOP_K)
    gate = g_p * e_p / jnp.sum(e_p, -1, keepdims=True)
    expert = g_idx * MOE_EXPERTS_PER_GROUP + e_idx
    return grouped_expert_ffn(h, expert, gate.astype(h.dtype), w13, w2)


def hybrid_layer(layer_idx, x, xc, c, c_ctx, cos, sin, mod_w, mod_b, mixer_params,
                 ln1_g, ln1_b, moe_params, ln2_g, ln2_b):
    B, L, D = x.shape
    C = xc.shape[1]
    sh1, sc1, g1, sh2, sc2, g2 = [m[:, None, :] for m in adaln_modulation(c, mod_w, mod_b)]
    sh1c, sc1c, g1c, sh2c, sc2c, g2c = adaln_modulation(c_ctx, mod_w, mod_b)
    u = x * (1.0 + sc1) + sh1
    uc = xc * (1.0 + sc1c) + sh1c
    kind = layer_idx % N_MIXERS
    if kind == 0:
        y, yc = neighbourhood_attention(u, uc, *mixer_params)
    elif kind == 1:
        y, yc = conformer_conv(u, uc, *mixer_params)
    elif kind == 2:
        y, yc = window_sink_attention(u, uc, *mixer_params, cos, sin)
    else:
        lambda_init = 0.8 - 0.6 * math.exp(-0.3 * layer_idx)
        y, yc = differential_attention(u, uc, *mixer_params, cos, sin, lambda_init)
    x = layer_norm(DEEPNORM_ALPHA * x + g1 * y, ln1_g, ln1_b)
    xc = layer_norm(DEEPNORM_ALPHA * xc + g1c * yc, ln1_g, ln1_b)
    u = x * (1.0 + sc2) + sh2
    uc = xc * (1.0 + sc2c) + sh2c
    tokens = jnp.concatenate([u.reshape(B * L, D), uc.reshape(B * C, D)], 0)
    f = hierarchical_moe(tokens, *moe_params)
    x = layer_norm(DEEPNORM_ALPHA * x + g2 * f[:B * L].reshape(B, L, D), ln2_g, ln2_b)
    xc = layer_norm(DEEPNORM_ALPHA * xc + g2c * f[B * L:].reshape(B, C, D), ln2_g, ln2_b)
    return x, xc


def setup_inputs(seed: int = 0) -> dict:
    key = jax.random.key(seed)
    keys = iter(jax.random.split(key, 32 * DEPTH + 8))
    D = D_MODEL

    def normal(shape, scale):
        return jax.random.normal(next(keys), shape, jnp.float32) * scale

    def gain(n):
        return 1.0 + normal((n,), 0.01)

    inputs = {
        'x': normal((BATCH, SEQ, D), 1.0),
        'c': normal((BATCH, D), 1.0),
        'ctx': normal((BATCH, CTX_LEN, D), 1.0),
        'c_ctx': normal((D,), 1.0),
    }
    for i in range(DEPTH):
        p = 'l%d_' % i
        inputs[p + 'mod_w'] = normal((D, 6 * D), D ** -0.5)
        inputs[p + 'mod_b'] = normal((6 * D,), 0.01)
        kind = i % N_MIXERS
        if kind == 0:
            inputs[p + 'na_w_qkv'] = normal((D, 3 * NA_HEADS * HEAD_DIM), D ** -0.5)
            inputs[p + 'na_rpb'] = normal((NA_HEADS, 2 * NA_KH - 1, 2 * NA_KW - 1), 0.02)
            inputs[p + 'na_w_o'] = normal((NA_HEADS * HEAD_DIM, D), (NA_HEADS * HEAD_DIM) ** -0.5 * DEEPNORM_BETA)
        elif kind == 1:
            inputs[p + 'cv_w_in'] = normal((D, 2 * D), D ** -0.5)
            inputs[p + 'cv_b_in'] = normal((2 * D,), 0.01)
            inputs[p + 'cv_dw'] = normal((CONV_WIDTH, D), CONV_WIDTH ** -0.5)
            inputs[p + 'cv_dw_b'] = normal((D,), 0.01)
            inputs[p + 'cv_ln_g'] = gain(D)
            inputs[p + 'cv_ln_b'] = normal((D,), 0.01)
            inputs[p + 'cv_w_out'] = normal((D, D), D ** -0.5 * DEEPNORM_BETA)
            inputs[p + 'cv_b_out'] = normal((D,), 0.01)
        elif kind == 2:
            inputs[p + 'sw_w_qkv'] = normal((D, (SWA_HEADS + 2 * SWA_KV_HEADS) * HEAD_DIM), D ** -0.5)
            inputs[p + 'sw_sink'] = normal((SWA_HEADS,), 0.5)
            inputs[p + 'sw_w_o'] = normal((SWA_HEADS * HEAD_DIM, D), (SWA_HEADS * HEAD_DIM) ** -0.5 * DEEPNORM_BETA)
        else:
            inputs[p + 'df_w_qkv'] = normal((D, 6 * DIFF_HEADS * HEAD_DIM), D ** -0.5)
            inputs[p + 'df_lambda'] = normal((4, HEAD_DIM), 0.1)
            inputs[p + 'df_subln_g'] = gain(2 * HEAD_DIM)
            inputs[p + 'df_w_o'] = normal((2 * DIFF_HEADS * HEAD_DIM, D), (2 * DIFF_HEADS * HEAD_DIM) ** -0.5 * DEEPNORM_BETA)
        inputs[p + 'ln1_g'] = gain(D)
        inputs[p + 'ln1_b'] = normal((D,), 0.01)
        inputs[p + 'router_g_w'] = normal((D, MOE_GROUPS), D ** -0.5)
        inputs[p + 'router_g_b'] = normal((MOE_GROUPS,), 0.01)
        inputs[p + 'router_e_w'] = normal((D, MOE_EXPERTS), D ** -0.5)
        inputs[p + 'router_e_b'] = normal((MOE_EXPERTS,), 0.01)
        inputs[p + 'moe_w13'] = normal((MOE_EXPERTS, D, 2 * MOE_D_FF), D ** -0.5)
        inputs[p + 'moe_w2'] = normal((MOE_EXPERTS, MOE_D_FF, D), MOE_D_FF ** -0.5 * DEEPNORM_BETA)
        inputs[p + 'ln2_g'] = gain(D)
        inputs[p + 'ln2_b'] = normal((D,), 0.01)
    return inputs


def reference(x, c, ctx, c_ctx,
              l0_mod_w, l0_mod_b, l0_na_w_qkv, l0_na_rpb, l0_na_w_o, l0_ln1_g, l0_ln1_b,
              l0_router_g_w, l0_router_g_b, l0_router_e_w, l0_router_e_b, l0_moe_w13, l0_moe_w2, l0_ln2_g, l0_ln2_b,
              l1_mod_w, l1_mod_b, l1_cv_w_in, l1_cv_b_in, l1_cv_dw, l1_cv_dw_b, l1_cv_ln_g, l1_cv_ln_b,
              l1_cv_w_out, l1_cv_b_out, l1_ln1_g, l1_ln1_b,
              l1_router_g_w, l1_router_g_b, l1_router_e_w, l1_router_e_b, l1_moe_w13, l1_moe_w2, l1_ln2_g, l1_ln2_b,
              l2_mod_w, l2_mod_b, l2_sw_w_qkv, l2_sw_sink, l2_sw_w_o, l2_ln1_g, l2_ln1_b,
              l2_router_g_w, l2_router_g_b, l2_router_e_w, l2_router_e_b, l2_moe_w13, l2_moe_w2, l2_ln2_g, l2_ln2_b,
              l3_mod_w, l3_mod_b, l3_df_w_qkv, l3_df_lambda, l3_df_subln_g, l3_df_w_o, l3_ln1_g, l3_ln1_b,
              l3_router_g_w, l3_router_g_b, l3_router_e_w, l3_router_e_b, l3_moe_w13, l3_moe_w2, l3_ln2_g, l3_ln2_b):
    layers = (
        (l0_mod_w, l0_mod_b, (l0_na_w_qkv, l0_na_rpb, l0_na_w_o), l0_ln1_g, l0_ln1_b,
         (l0_router_g_w, l0_router_g_b, l0_router_e_w, l0_router_e_b, l0_moe_w13, l0_moe_w2), l0_ln2_g, l0_ln2_b),
        (l1_mod_w, l1_mod_b, (l1_cv_w_in, l1_cv_b_in, l1_cv_dw, l1_cv_dw_b, l1_cv_ln_g, l1_cv_ln_b, l1_cv_w_out, l1_cv_b_out),
         l1_ln1_g, l1_ln1_b,
         (l1_router_g_w, l1_router_g_b, l1_router_e_w, l1_router_e_b, l1_moe_w13, l1_moe_w2), l1_ln2_g, l1_ln2_b),
        (l2_mod_w, l2_mod_b, (l2_sw_w_qkv, l2_sw_sink, l2_sw_w_o), l2_ln1_g, l2_ln1_b,
         (l2_router_g_w, l2_router_g_b, l2_router_e_w, l2_router_e_b, l2_moe_w13, l2_moe_w2), l2_ln2_g, l2_ln2_b),
        (l3_mod_w, l3_mod_b, (l3_df_w_qkv, l3_df_lambda, l3_df_subln_g, l3_df_w_o), l3_ln1_g, l3_ln1_b,
         (l3_router_g_w, l3_router_g_b, l3_router_e_w, l3_router_e_b, l3_moe_w13, l3_moe_w2), l3_ln2_g, l3_ln2_b),
    )
    cos, sin = axial_rope_tables(x.shape[1])
    xc = ctx
    for i in range(DEPTH):
        x, xc = hybrid_layer(i, x, xc, c, c_ctx, cos, sin, *layers[i])
    return x
```

```python
import math
from contextlib import ExitStack
import numpy as np
import concourse.bass as bass
import concourse.mybir as mybir
from concourse.bass_utils import run_bass_kernel_spmd

F32 = mybir.dt.float32
BF16 = mybir.dt.bfloat16
I32 = mybir.dt.int32
AF = mybir.ActivationFunctionType
ALU = mybir.AluOpType
AX = mybir.AxisListType

D = 2048
L = 2048
C = 256
T = L + C
NT = T // 128
KC = 16
DEPTH = 4
ALPHA = (2.0 * DEPTH) ** 0.25
EPS = 1e-5
EPSP = EPS / (ALPHA * ALPHA)
SCALE = 128 ** -0.5
NEG = -30000.0
BLK = 256
NBLK = (2 * T) // BLK + 32
NSLOT = NBLK * BLK
ND = 40
TB256 = [(i * 256, 256, 0 if i < 8 else 1) for i in range(9)]
TB512 = [(i * 512, 512, 0) for i in range(4)] + [(2048, 256, 1)]


class Bld:
    def __init__(self):
        self.nc = bass.Bass("TRN2", target_bir_lowering=False)
        nc = self.nc
        self.es = ExitStack()
        self.eng = {"pe": nc.tensor, "act": nc.scalar, "dve": nc.vector, "pool": nc.gpsimd, "sp": nc.sync}
        self.esem = {e: self.es.enter_context(nc.semaphore("s_" + e)) for e in self.eng}
        self.ecnt = {e: 0 for e in self.eng}
        self.dsem = [self.es.enter_context(nc.semaphore("d%d" % i)) for i in range(ND)]
        self.dcnt = [0] * ND
        self.dnext = 0
        self.seen = {e: {} for e in self.eng}
        self.lastw = {}
        self.readers = {}
        self.uid = 0

    def _waits(self, e, reads, writes):
        evs = {}
        for k in reads:
            ev = self.lastw.get(k)
            if ev is not None:
                evs[ev[0]] = max(evs.get(ev[0], 0), ev[1])
        for k in writes:
            ev = self.lastw.get(k)
            if ev is not None:
                evs[ev[0]] = max(evs.get(ev[0], 0), ev[1])
            for s, v in self.readers.get(k, {}).items():
                evs[s] = max(evs.get(s, 0), v)
        for s, v in evs.items():
            if s == ("e", "pe") and e == "pe":
                continue
            if self.seen[e].get(s, 0) >= v:
                continue
            self.seen[e][s] = v
            sem = self.esem[s[1]] if s[0] == "e" else self.dsem[s[1]]
            self.eng[e].wait_ge(sem, v)

    def _commit(self, ev, reads, writes):
        for k in writes:
            self.lastw[k] = ev
            self.readers[k] = {}
        for k in reads:
            r = self.readers.setdefault(k, {})
            r[ev[0]] = max(r.get(ev[0], 0), ev[1])

    def op(self, e, fn, reads=(), writes=()):
        self._waits(e, reads, writes)
        self.ecnt[e] += 1
        fn(self.eng[e]).then_inc(self.esem[e], 1)
        self._commit((("e", e), self.ecnt[e]), reads, writes)

    def dma(self, q, fn, reads=(), writes=()):
        i = self.dnext
        self.dnext = (i + 1) % ND
        prev = self.dcnt[i]
        s = ("d", i)
        if prev > 0 and self.seen[q].get(s, 0) < prev:
            self.eng[q].wait_ge(self.dsem[i], prev)
            self.seen[q][s] = prev
        self._waits(q, reads, writes)
        self.dcnt[i] += 16
        fn(self.eng[q]).then_inc(self.dsem[i], 16)
        self._commit((s, self.dcnt[i]), reads, writes)

    def barrier(self):
        for e in self.eng:
            for e2 in self.eng:
                if e2 != e and self.ecnt[e2] > self.seen[e].get(("e", e2), 0):
                    self.eng[e].wait_ge(self.esem[e2], self.ecnt[e2])
                    self.seen[e][("e", e2)] = self.ecnt[e2]
            for i in range(ND):
                if self.dcnt[i] > self.seen[e].get(("d", i), 0):
                    self.eng[e].wait_ge(self.dsem[i], self.dcnt[i])
                    self.seen[e][("d", i)] = self.dcnt[i]
        for e in self.eng:
            if self.ecnt[e] > 0:
                self.eng[e].wait_ge(self.esem[e], self.ecnt[e])

    def sb(self, stack, name, shape, dt):
        self.uid += 1
        return stack.enter_context(self.nc.sbuf_tensor("%s_%d" % (name, self.uid), shape, dt))

    def dram(self, name, shape, dt, kind="Internal"):
        return self.nc.dram_tensor(name, shape, dt, kind=kind).ap()


def vec_layout(kind):
    cols = {}
    n = 0

    def add(name, w):
        nonlocal n
        cols[name] = n
        n += w

    add("mod_b", 96)
    add("ln1_g", 16)
    add("ln1_b", 16)
    add("ln2_g", 16)
    add("ln2_b", 16)
    add("rb", 36)
    if kind == 1:
        add("b_in", 32)
        add("dw", 31 * 16)
        add("dw_b", 16)
        add("cln_g", 16)
        add("cln_b", 16)
        add("b_out", 16)
    if kind == 2:
        add("sink", 16)
    if kind == 3:
        add("subg", 2)
        add("lam", 4)
    return cols, n


def pk(v):
    v = np.asarray(v, np.float32)
    return np.ascontiguousarray(v.reshape(-1, 128).T)


def build(dbg=None):
    b = Bld()
    nc = b.nc
    G = ExitStack()
    okind = "ExternalOutput"
    skind = "ExternalOutput" if dbg else "Internal"

    xT_in = b.dram("xT", [D, T], F32, "ExternalInput")
    cT_in = b.dram("cT", [128, KC, 2], F32, "ExternalInput")
    rope_in = b.dram("rope", [2, 128, T], F32, "ExternalInput")
    cst_in = b.dram("cst", [128, 4, 128], F32, "ExternalInput")
    cst2_in = b.dram("cst2", [128, 66], F32, "ExternalInput")
    swm_in = b.dram("swm", [6, 128, 512], F32, "ExternalInput")
    nab_in = b.dram("nab", [16, 20, 128, 512], F32, "ExternalInput")
    vec_in, mod_w, rw_in, w13_in, w2_in = [], [], [], [], []
    VL = [vec_layout(i) for i in range(4)]
    for i in range(4):
        vec_in.append(b.dram("vec%d" % i, [128, VL[i][1]], F32, "ExternalInput"))
        mod_w.append(b.dram("modw%d" % i, [D, 6 * D], F32, "ExternalInput"))
        rw_in.append(b.dram("rw%d" % i, [128, KC, 36], F32, "ExternalInput"))
        w13_in.append([b.dram("w13_%d_%d" % (i, m), [32 * 128, 4096], F32, "ExternalInput") for m in range(4)])
        w2_in.append([b.dram("w2_%d_%d" % (i, m), [32 * 128, 2048], F32, "ExternalInput") for m in range(4)])
    wqkv0 = b.dram("wqkv0", [D, 6144], F32, "ExternalInput")
    wo0 = b.dram("wo0", [D, D], F32, "ExternalInput")
    win1 = b.dram("win1", [D, 4096], F32, "ExternalInput")
    wo1 = b.dram("wo1", [D, D], F32, "ExternalInput")
    wqkv2 = b.dram("wqkv2", [D, 3072], F32, "ExternalInput")
    wo2 = b.dram("wo2", [D, D], F32, "ExternalInput")
    wqkv3 = b.dram("wqkv3", [D, 6144], F32, "ExternalInput")
    wo3 = b.dram("wo3", [D, D], F32, "ExternalInput")
    out_T = b.dram("outT", [D, L], F32, okind)

    XA = b.dram("XA", [D, T], F32, skind)
    XB = b.dram("XB", [D, T], F32, skind)
    QT = b.dram("QT", [D, T], BF16, skind)
    KT = b.dram("KT", [D, T], BF16, skind)
    VV = b.dram("VV", [T, D], BF16, skind)
    OT = b.dram("OT", [D, T], BF16, skind)
    CT = b.dram("CT", [D, T], F32, skind)
    HTOK = b.dram("HTOK", [T, D], BF16, skind)
    XS = b.dram("XS", [NSLOT, D], BF16, skind)
    YS = b.dram("YS", [NSLOT, D], BF16, skind)
    RDBG = b.dram("RDBG", [128, NT * 36], F32, skind)

    cst = b.sb(G, "cst", [128, 4, 128], F32)
    cst2 = b.sb(G, "cst2", [128, 66], F32)
    utb = b.sb(G, "utb", [128, 128], BF16)
    identf = cst[:, 0, :]
    identb = b.sb(G, "identb", [128, 128], BF16)
    onesb = b.sb(G, "onesb", [128, 128], BF16)
    permb = b.sb(G, "permb", [128, 128], BF16)
    modv = b.sb(G, "modv", [128, 4, 6, 16, 2], F32)
    vecs = [b.sb(G, "vec%d" % i, [128, VL[i][1]], F32) for i in range(4)]
    der = b.sb(G, "der", [128, 6, 16, 2], F32)
    tmpd = b.sb(G, "tmpd", [128, 16, 2], F32)
    lg = b.sb(G, "lg", [128, NT, 36], F32)
    slot_i = b.sb(G, "slot_i", [128, 2, NT], I32)
    gate = b.sb(G, "gate", [128, 2, NT], F32)
    widx = b.sb(G, "widx", [128, NBLK], I32)
    PS = [G.enter_context(nc.psum_tensor("ps%d" % i, [128, 512], F32)) for i in range(8)]
    PK = ["ps%d" % i for i in range(8)]

    b.dma("sp", lambda q: q.dma_start(out=cst[:], in_=cst_in), writes=["cst"])
    for i in range(4):
        b.dma("sp", lambda q, i=i: q.dma_start(out=vecs[i][:], in_=vec_in[i]), writes=["vec%d" % i])
    b.op("dve", lambda e: e.tensor_copy(out=identb[:], in_=cst[:, 0, :]), reads=["cst"], writes=["identb"])
    b.op("dve", lambda e: e.tensor_copy(out=onesb[:], in_=cst[:, 1, :]), reads=["cst"], writes=["onesb"])
    b.op("dve", lambda e: e.tensor_copy(out=permb[:], in_=cst[:, 2, :]), reads=["cst"], writes=["permb"])
    b.op("dve", lambda e: e.tensor_copy(out=utb[:], in_=cst[:, 3, :]), reads=["cst"], writes=["utb"])
    b.dma("sp", lambda q: q.dma_start(out=cst2[:], in_=cst2_in), writes=["cst2"])

    def vcol(i, name, c=0, w=1):
        o = VL[i][0][name] + c
        return vecs[i][:, o:o + w]

    def phase_mod():
        with ExitStack() as S:
            mw = [b.sb(S, "mw", [128, KC, 1024], BF16) for _ in range(2)]
            cT = b.sb(S, "cT", [128, KC, 2], F32)
            sc = b.sb(S, "sc", [128, KC, 2], BF16)
            b.dma("sp", lambda q: q.dma_start(out=cT[:], in_=cT_in), writes=["cT"])
            b.op("act", lambda e: e.activation(out=sc[:], in_=cT[:], func=AF.Silu), reads=["cT"], writes=["sc"])
            n = 0
            for li in range(4):
                src = mod_w[li].rearrange("(c p) n -> p c n", p=128)
                for pc in range(12):
                    buf = mw[n % 2]
                    bk = "mw%d" % (n % 2)
                    n += 1
                    b.dma("pool", lambda q, buf=buf, pc=pc, src=src: q.dma_start(out=buf[:], in_=src[:, :, pc * 1024:(pc + 1) * 1024]),
                          writes=[bk])
                    for j in range(8):
                        oc = pc * 8 + j
                        pi = oc % 2
                        for k in range(KC):
                            b.op("pe", lambda e, k=k, j=j, buf=buf, pi=pi: e.matmul(PS[pi][:, 0:2], lhsT=buf[:, k, j * 128:(j + 1) * 128],
                                                                                      rhs=sc[:, k, :], start=(k == 0), stop=(k == KC - 1)),
                                 reads=[bk, "sc"], writes=[PK[pi]])
                        b.op("dve", lambda e, li=li, oc=oc, pi=pi: e.tensor_scalar(out=modv[:, li, oc // 16, oc % 16, :], in0=PS[pi][:, 0:2],
                                                                                    scalar1=vcol(li, "mod_b", oc), scalar2=None, op0=ALU.add),
                             reads=[PK[pi], "vec%d" % li], writes=["modv"])
        b.barrier()

    def derive(li):
        m = lambda k: modv[:, li, k]
        R = ["modv", "vec%d" % li]
        b.op("dve", lambda e: e.tensor_scalar(out=der[:, 0], in0=m(1), scalar1=1.0, scalar2=None, op0=ALU.add), reads=R, writes=["der"])
        b.op("dve", lambda e: e.tensor_copy(out=der[:, 1], in_=m(0)), reads=R, writes=["der"])
        b.op("dve", lambda e: e.tensor_scalar(out=der[:, 2], in0=m(2), scalar1=1.0 / ALPHA, scalar2=None, op0=ALU.mult), reads=R, writes=["der"])
        b.op("dve", lambda e: e.tensor_scalar(out=tmpd[:], in0=m(4), scalar1=1.0, scalar2=None, op0=ALU.add), reads=R, writes=["tmpd"])
        for j in range(2):
            b.op("dve", lambda e, j=j: e.tensor_tensor(out=der[:, 3, :, j], in0=tmpd[:, :, j], in1=vcol(li, "ln1_g", 0, 16), op=ALU.mult),
                 reads=R + ["tmpd"], writes=["der"])
            b.op("dve", lambda e, j=j: e.tensor_tensor(out=der[:, 4, :, j], in0=tmpd[:, :, j], in1=vcol(li, "ln1_b", 0, 16), op=ALU.mult),
                 reads=R + ["tmpd"], writes=["der"])
        b.op("dve", lambda e: e.tensor_tensor(out=der[:, 4], in0=der[:, 4], in1=m(3), op=ALU.add), reads=R + ["der"], writes=["der"])
        b.op("dve", lambda e: e.tensor_scalar(out=der[:, 5], in0=m(5), scalar1=1.0 / ALPHA, scalar2=None, op0=ALU.mult), reads=R, writes=["der"])

    def build_u(S, XT):
        uT = b.sb(S, "uT", [128, KC, T], BF16)
        xs_ = [b.sb(S, "xst", [128, KC, 256], F32) for _ in range(2)]
        src = XT.rearrange("(c p) t -> p c t", p=128)
        for n, (t0, tn, j) in enumerate(TB256):
            xb = xs_[n % 2]
            kx = "xst%d" % (n % 2)
            b.dma("sp", lambda q, xb=xb, t0=t0: q.dma_start(out=xb[:], in_=src[:, :, t0:t0 + 256]), writes=[kx])
            for c in range(KC):
                b.op("act", lambda e, xb=xb, c=c, t0=t0, j=j: e.activation(out=uT[:, c, t0:t0 + 256], in_=xb[:, c, :], func=AF.Identity,
                                                                       scale=der[:, 0, c, j:j + 1], bias=der[:, 1, c, j:j + 1]),
                     reads=[kx, "der"], writes=["uT"])
        return uT

    def proj_fm(S, uT, W, oc0, noc, dstT, rope):
        wq = [b.sb(S, "wq", [128, KC, 512], BF16) for _ in range(2)]
        stg = [b.sb(S, "qstg", [128, T], BF16) for _ in range(2)]
        if rope:
            rp = b.sb(S, "rp", [128, 2, T], F32)
            b.dma("sp", lambda q: q.dma_start(out=rp[:], in_=rope_in.rearrange("a p t -> p a t")), writes=["rp"])
            qb = [b.sb(S, "qb", [128, 512], BF16) for _ in range(2)]
            t1 = [b.sb(S, "t1", [128, 512], F32) for _ in range(2)]
            t2 = [b.sb(S, "t2", [128, 512], F32) for _ in range(2)]
        src = W.rearrange("(c p) n -> p c n", p=128)
        npc = (noc + 3) // 4
        cnt = 0
        for pc in range(npc):
            wb = wq[pc % 2]
            kw = "wq%d" % (pc % 2)
            c0 = (oc0 + pc * 4) * 128
            ncol = min(4, noc - pc * 4) * 128
            b.dma("pool", lambda q, wb=wb, c0=c0, ncol=ncol: q.dma_start(out=wb[:, :, 0:ncol], in_=src[:, :, c0:c0 + ncol]), writes=[kw])
            for jj in range(ncol // 128):
                oc = pc * 4 + jj
                sg = stg[oc % 2]
                ks = "qstg%d" % (oc % 2)
                for (t0, tn, _) in TB512:
                    pi = cnt % 2
                    r = cnt % 2
                    cnt += 1
                    for k in range(KC):
                        b.op("pe", lambda e, k=k, jj=jj, wb=wb, pi=pi, t0=t0, tn=tn: e.matmul(PS[pi][:, 0:tn], lhsT=wb[:, k, jj * 128:(jj + 1) * 128],
                                                                                              rhs=uT[:, k, t0:t0 + tn], start=(k == 0), stop=(k == KC - 1)),
                             reads=[kw, "uT"], writes=[PK[pi]])
                    if not rope:
                        b.op("act", lambda e, sg=sg, pi=pi, t0=t0, tn=tn: e.activation(out=sg[:, t0:t0 + tn], in_=PS[pi][:, 0:tn], func=AF.Copy),
                             reads=[PK[pi]], writes=[ks])
                    else:
                        b.op("act", lambda e, r=r, pi=pi, tn=tn: e.activation(out=qb[r][:, 0:tn], in_=PS[pi][:, 0:tn], func=AF.Copy),
                             reads=[PK[pi]], writes=["qb%d" % r])
                        b.op("pe", lambda e, r=r, tn=tn: e.matmul(PS[2 + r][:, 0:tn], lhsT=permb[:], rhs=qb[r][:, 0:tn], start=True, stop=True),
                             reads=["permb", "qb%d" % r], writes=[PK[2 + r]])
                        b.op("dve", lambda e, r=r, t0=t0, tn=tn: e.tensor_tensor(out=t1[r][:, 0:tn], in0=qb[r][:, 0:tn], in1=rp[:, 0, t0:t0 + tn], op=ALU.mult),
                             reads=["qb%d" % r, "rp"], writes=["t1%d" % r])
                        b.op("dve", lambda e, r=r, t0=t0, tn=tn: e.tensor_tensor(out=t2[r][:, 0:tn], in0=PS[2 + r][:, 0:tn], in1=rp[:, 1, t0:t0 + tn], op=ALU.mult),
                             reads=[PK[2 + r], "rp"], writes=["t2%d" % r])
                        b.op("dve", lambda e, r=r, sg=sg, t0=t0, tn=tn: e.tensor_tensor(out=sg[:, t0:t0 + tn], in0=t1[r][:, 0:tn], in1=t2[r][:, 0:tn], op=ALU.add),
                             reads=["t1%d" % r, "t2%d" % r], writes=[ks])
                b.dma("sp", lambda q, sg=sg, oc=oc: q.dma_start(out=dstT[oc * 128:(oc + 1) * 128, :], in_=sg[:]), reads=[ks], writes=[("dstT", id(dstT))])

    def proj_tm(S, uT, W, c0, ncols, dst):
        wq = [b.sb(S, "wv", [128, KC, 512], BF16) for _ in range(2)]
        stg = [b.sb(S, "vstg", [128, 512], BF16) for _ in range(3)]
        src = W.rearrange("(c p) n -> p c n", p=128)
        cnt = 0
        for pc in range(ncols // 512):
            wb = wq[pc % 2]
            kw = "wv%d" % (pc % 2)
            b.dma("pool", lambda q, wb=wb, pc=pc: q.dma_start(out=wb[:], in_=src[:, :, c0 + pc * 512:c0 + (pc + 1) * 512]), writes=[kw])
            for i in range(NT):
                pi = 4 + cnt % 2
                sg = stg[cnt % 3]
                ks = "vstg%d" % (cnt % 3)
                cnt += 1
                for k in range(KC):
                    b.op("pe", lambda e, k=k, wb=wb, pi=pi, i=i: e.matmul(PS[pi][:], lhsT=uT[:, k, i * 128:(i + 1) * 128], rhs=wb[:, k, :],
                                                                           start=(k == 0), stop=(k == KC - 1)),
                         reads=[kw, "uT"], writes=[PK[pi]])
                b.op("act", lambda e, sg=sg, pi=pi: e.activation(out=sg[:], in_=PS[pi][:], func=AF.Copy), reads=[PK[pi]], writes=[ks])
                b.dma("sp", lambda q, sg=sg, i=i, pc=pc: q.dma_start(out=dst[i * 128:(i + 1) * 128, pc * 512:(pc + 1) * 512], in_=sg[:]),
                      reads=[ks], writes=[("dst", id(dst))])

    def phase_premix_attn(li, XT, W, nq, nk, nv, rope):
        with ExitStack() as S:
            uT = build_u(S, XT)
            with ExitStack() as S2:
                proj_fm(S2, uT, W, 0, nq, QT, rope)
            b.barrier()
            with ExitStack() as S2:
                proj_fm(S2, uT, W, nq, nk, KT, rope)
            b.barrier()
            with ExitStack() as S2:
                proj_tm(S2, uT, W, (nq + nk) * 128, nv, VV)
        b.barrier()

    def attn_head_loop(S, heads, Dv, qblocks_fn, finish_fn, extra_den=None):
        nd = Dv // 128
        qh = [b.sb(S, "qh", [128, T], BF16) for _ in range(2)]
        kh = [b.sb(S, "kh", [128, T], BF16) for _ in range(2)]
        vh = [b.sb(S, "vh", [128, NT, Dv], BF16) for _ in range(2)]
        pt = [b.sb(S, "pt", [128, 512], BF16) for _ in range(4)]
        pt0 = [b.sb(S, "pt0", [128, 512], BF16) for _ in range(2)]
        sb_ = [b.sb(S, "sbias", [128, 512], F32) for _ in range(2)]
        bt = [b.sb(S, "btile", [128, 512], F32) for _ in range(4)]
        rec = [b.sb(S, "rec", [128, 512], F32) for _ in range(2)]
        swm = b.sb(S, "swm", [128, 6, 512], BF16)
        b.dma("pool", lambda q: q.dma_start(out=swm[:], in_=swm_in.rearrange("a p t -> p a t")), writes=["swm"])
        vsrc = VV.rearrange("(n p) d -> p n d", p=128)
        ACC = [([2, 3], 4), ([5, 6], 7)]
        items = []
        qbn = 0
        for hn, (qrow, krow, vcol_, tag) in enumerate(heads):
            for (q0, nq, chunks) in qblocks_fn(tag):
                for ci, ch in enumerate(chunks):
                    items.append(dict(hn=hn, qrow=qrow, krow=krow, vcol=vcol_, tag=tag, q0=q0, nq=nq, ci=ci, nch=len(chunks), ch=ch,
                                      first=(q0 == 0 and ci == 0), acc=qbn % 2))
                qbn += 1
        cnt = {"s": 0, "p": 0, "b": 0}

        def stageA(it):
            hn = it["hn"]
            r2 = hn % 2
            q0, nq = it["q0"], it["nq"]
            k0, kind, arg = it["ch"]
            si = cnt["s"] % 2
            cnt["s"] += 1
            b.op("pe", lambda e: e.matmul(PS[si][:, 0:nq], lhsT=kh[r2][:, k0:k0 + 128], rhs=qh[r2][:, q0:q0 + nq], start=True, stop=True),
                 reads=["qh%d" % r2, "kh%d" % r2], writes=[PK[si]])
            pi = cnt["p"] % 4
            cnt["p"] += 1
            P = pt[pi]
            kp = "pt%d" % pi
            it["P"], it["kp"] = P, kp
            if kind == "bias":
                bi = cnt["b"] % 4
                bj = cnt["b"] % 2
                cnt["b"] += 1
                b.dma("sp", lambda q: q.dma_start(out=bt[bi][:], in_=nab_in[arg[0], arg[1]]), writes=["bt%d" % bi])
                b.op("dve", lambda e: e.scalar_tensor_tensor(out=sb_[bj][:, 0:nq], in0=PS[si][:, 0:nq], scalar=SCALE, in1=bt[bi][:, 0:nq], op0=ALU.mult, op1=ALU.add),
                     reads=[PK[si], "bt%d" % bi], writes=["sb%d" % bj])
                b.op("act", lambda e: e.activation(out=P[:, 0:nq], in_=sb_[bj][:, 0:nq], func=AF.Exp), reads=["sb%d" % bj], writes=[kp])
            elif kind == "mask":
                bj = cnt["b"] % 2
                cnt["b"] += 1
                b.op("act", lambda e: e.activation(out=pt0[bj][:, 0:nq], in_=PS[si][:, 0:nq], func=AF.Exp, scale=SCALE), reads=[PK[si]], writes=["pt0%d" % bj])
                b.op("dve", lambda e: e.tensor_tensor(out=P[:, 0:nq], in0=pt0[bj][:, 0:nq], in1=swm[:, arg, 0:nq], op=ALU.mult), reads=["pt0%d" % bj, "swm"], writes=[kp])
            else:
                b.op("act", lambda e: e.activation(out=P[:, 0:nq], in_=PS[si][:, 0:nq], func=AF.Exp, scale=SCALE), reads=[PK[si]], writes=[kp])

        def stageB(it):
            hn = it["hn"]
            r2 = hn % 2
            q0, nq, ci, nch = it["q0"], it["nq"], it["ci"], it["nch"]
            P, kp = it["P"], it["kp"]
            ob, rb = ACC[it["acc"]]
            kt = it["ch"][0] // 128
            for dv in range(nd):
                b.op("pe", lambda e, dv=dv: e.matmul(PS[ob[dv]][:, 0:nq], lhsT=vh[r2][:, kt, dv * 128:(dv + 1) * 128], rhs=P[:, 0:nq], start=(ci == 0), stop=(ci == nch - 1)),
                     reads=[kp, "vh%d" % r2], writes=[PK[ob[dv]]])
            b.op("pe", lambda e: e.matmul(PS[rb][:, 0:nq], lhsT=onesb[:], rhs=P[:, 0:nq], start=(ci == 0), stop=(ci == nch - 1)), reads=[kp, "onesb"], writes=[PK[rb]])
            if ci == nch - 1:
                ri = it["acc"]
                tag = it["tag"]
                if extra_den is not None:
                    b.op("dve", lambda e: e.tensor_scalar(out=rec[ri][:, 0:nq], in0=PS[rb][:, 0:nq], scalar1=extra_den(tag), scalar2=None, op0=ALU.add),
                         reads=[PK[rb], "esink"], writes=["rec%d" % ri])
                    b.op("dve", lambda e: e.reciprocal(out=rec[ri][:, 0:nq], in_=rec[ri][:, 0:nq]), reads=["rec%d" % ri], writes=["rec%d" % ri])
                else:
                    b.op("dve", lambda e: e.reciprocal(out=rec[ri][:, 0:nq], in_=PS[rb][:, 0:nq]), reads=[PK[rb]], writes=["rec%d" % ri])
                finish_fn(hn, tag, q0, nq, rec[ri], "rec%d" % ri, ob, rb)

        def load_head(hn):
            if hn >= len(heads):
                return
            qrow, krow, vcol_, tag = heads[hn]
            r2 = hn % 2
            b.dma("sp", lambda q: q.dma_start(out=qh[r2][:], in_=QT[qrow:qrow + 128, :]), reads=[("dstT", id(QT))], writes=["qh%d" % r2])
            b.dma("sp", lambda q: q.dma_start(out=kh[r2][:], in_=KT[krow:krow + 128, :]), reads=[("dstT", id(KT))], writes=["kh%d" % r2])
            b.dma("sp", lambda q: q.dma_start(out=vh[r2][:], in_=vsrc[:, :, vcol_:vcol_ + Dv]), reads=[("dst", id(VV))], writes=["vh%d" % r2])

        load_head(0)
        load_head(1)
        LOOK = 2
        for i in range(len(items) + LOOK):
            if i < len(items):
                stageA(items[i])
            if i - LOOK >= 0:
                itb = items[i - LOOK]
                stageB(itb)
                if i - LOOK + 1 == len(items) or items[i - LOOK + 1]["hn"] != itb["hn"]:
                    load_head(itb["hn"] + 2)

    def simple_finish(S):
        ostg = [b.sb(S, "ostg", [128, T], BF16) for _ in range(2)]

        def fin(hn, tag, q0, nq, rec, krec, ob, rb):
            og = ostg[hn % 2]
            ko = "ostg%d" % (hn % 2)
            b.op("dve", lambda e: e.tensor_tensor(out=og[:, q0:q0 + nq], in0=PS[ob[0]][:, 0:nq], in1=rec[:, 0:nq], op=ALU.mult),
                 reads=[PK[ob[0]], krec], writes=[ko])
            if q0 + nq == T:
                b.dma("sp", lambda q: q.dma_start(out=OT[tag * 128:(tag + 1) * 128, :], in_=og[:]), reads=[ko], writes=[("dstT", id(OT))])
        return fin

    def na_qblocks(h):
        res = []
        for bq in range(4):
            if bq == 0:
                kcs = [(kc, kc) for kc in range(6)]
            elif bq == 3:
                kcs = [(kc, 14 + kc - 10) for kc in range(10, 16)]
            else:
                kcs = [(4 * bq - 2 + j, 6 + j) for j in range(8)]
            ch = [(kc * 128, "bias", (h, ti)) for kc, ti in kcs]
            ch += [(2048, "none", None), (2176, "none", None)]
            res.append((bq * 512, 512, ch))
        res.append((2048, 256, [(2048, "none", None), (2176, "none", None)]))
        return res

    def phase_attn_na():
        with ExitStack() as S:
            heads = [(h * 128, h * 128, h * 128, h) for h in range(16)]
            attn_head_loop(S, heads, 128, na_qblocks, simple_finish(S))
        b.barrier()

    def ln_phase(li, XT, XTn, y_src, gidx, lng, lnb, with_router, final=False):
        with ExitStack() as S:
            xz = [b.sb(S, "xz", [128, KC, 256], F32) for _ in range(2)]
            x1 = [b.sb(S, "x1", [128, KC, 256], F32) for _ in range(2)]
            zb = [b.sb(S, "zb", [128, 256], BF16) for _ in range(3)]
            zq = [b.sb(S, "zq", [128, 256], BF16) for _ in range(3)]
            mean = b.sb(S, "mean", [128, 256], F32)
            rstd = b.sb(S, "rstd", [128, 256], F32)
            var = b.sb(S, "var", [128, 256], F32)
            hst = [b.sb(S, "hst", [128, 512], BF16) for _ in range(3)]
            if with_router:
                rw = b.sb(S, "rw", [128, KC, 36], F32)
                b.dma("sp", lambda q: q.dma_start(out=rw[:], in_=rw_in[li]), writes=["rw"])
            zfn = y_src(S)
            src = XT.rearrange("(c p) t -> p c t", p=128)
            dstv = XTn.rearrange("(c p) t -> p c t", p=128) if not final else out_T.rearrange("(c p) t -> p c t", p=128)
            cnt = {"z": 0, "h": 0}
            pend = {"stats": None, "tr": None}
            SBK = [(2, 3), (6, 7)]

            def loadX(tbi):
                t0 = TB256[tbi][0]
                X = xz[tbi % 2]
                b.dma("sp", lambda q: q.dma_start(out=X[:], in_=src[:, :, t0:t0 + 256]), reads=[("xt", id(XT))], writes=["xz%d" % (tbi % 2)])

            def P1chunk(tbi, c):
                t0, tn, j = TB256[tbi]
                X = xz[tbi % 2]
                kx = "xz%d" % (tbi % 2)
                s1, s2 = SBK[tbi % 2]
                zfn(tbi, t0, j, c, X, kx)
                zi = cnt["z"] % 3
                cnt["z"] += 1
                b.op("act", lambda e: e.activation(out=zb[zi][:], in_=X[:, c, :], func=AF.Copy), reads=[kx], writes=["zb%d" % zi])
                b.op("act", lambda e: e.activation(out=zq[zi][:], in_=X[:, c, :], func=AF.Square), reads=[kx], writes=["zq%d" % zi])
                def stats(c=c, zi=zi, s1=s1, s2=s2):
                    b.op("pe", lambda e: e.matmul(PS[s1][:, 0:256], lhsT=onesb[:], rhs=zb[zi][:], start=(c == 0), stop=(c == KC - 1)),
                         reads=["zb%d" % zi, "onesb"], writes=[PK[s1]])
                    b.op("pe", lambda e: e.matmul(PS[s2][:, 0:256], lhsT=onesb[:], rhs=zq[zi][:], start=(c == 0), stop=(c == KC - 1)),
                         reads=["zq%d" % zi, "onesb"], writes=[PK[s2]])
                if pend["stats"] is not None:
                    pend["stats"]()
                pend["stats"] = stats
                if c == KC - 1:
                    pend["stats"]()
                    pend["stats"] = None

            def P2pre(tbi):
                s1, s2 = SBK[tbi % 2]
                b.op("dve", lambda e: e.tensor_scalar(out=mean[:], in0=PS[s1][:, 0:256], scalar1=1.0 / D, scalar2=None, op0=ALU.mult), reads=[PK[s1]], writes=["mean"])
                b.op("dve", lambda e: e.tensor_tensor(out=var[:], in0=mean[:], in1=mean[:], op=ALU.mult), reads=["mean"], writes=["var"])
                b.op("dve", lambda e: e.scalar_tensor_tensor(out=var[:], in0=PS[s2][:, 0:256], scalar=1.0 / D, in1=var[:], op0=ALU.mult, op1=ALU.subtract),
                     reads=[PK[s2], "var"], writes=["var"])
                b.op("dve", lambda e: e.tensor_scalar(out=var[:], in0=var[:], scalar1=EPSP, scalar2=None, op0=ALU.add), reads=["var"], writes=["var"])
                b.op("act", lambda e: e.activation(out=var[:], in_=var[:], func=AF.Sqrt), reads=["var"], writes=["var"])
                b.op("dve", lambda e: e.reciprocal(out=rstd[:], in_=var[:]), reads=["var"], writes=["rstd"])

            def P2chunk(tbi, c):
                t0, tn, j = TB256[tbi]
                X = xz[tbi % 2]
                kx = "xz%d" % (tbi % 2)
                X1 = x1[tbi % 2]
                k1 = "x1%d" % (tbi % 2)
                b.op("dve", lambda e: e.tensor_tensor(out=X[:, c, :], in0=X[:, c, :], in1=mean[:], op=ALU.subtract), reads=[kx, "mean"], writes=[kx])
                b.op("dve", lambda e: e.tensor_tensor(out=X[:, c, :], in0=X[:, c, :], in1=rstd[:], op=ALU.mult), reads=[kx, "rstd"], writes=[kx])
                b.op("act", lambda e: e.activation(out=X1[:, c, :], in_=X[:, c, :], func=AF.Identity, scale=vcol(li, lng, c), bias=vcol(li, lnb, c)),
                     reads=[kx, "vec%d" % li], writes=[k1])
                if with_router:
                    b.op("act", lambda e: e.activation(out=X[:, c, :], in_=X[:, c, :], func=AF.Identity, scale=der[:, 3, c, j:j + 1], bias=der[:, 4, c, j:j + 1]),
                         reads=[kx, "der"], writes=[kx])
                    def trgroup(g4=c // 4, X=X, kx=kx, tbi=tbi):
                        for tt in range(2):
                            ti = tbi * 2 + tt
                            hi = cnt["h"] % 3
                            cnt["h"] += 1
                            for cc in range(4):
                                c2 = g4 * 4 + cc
                                b.op("pe", lambda e, c2=c2, cc=cc, tt=tt: e.transpose(PS[5][:, cc * 128:(cc + 1) * 128], X[:, c2, tt * 128:(tt + 1) * 128], identf),
                                     reads=[kx, "cst"], writes=[PK[5]])
                            b.op("act", lambda e, hi=hi: e.activation(out=hst[hi][:], in_=PS[5][:], func=AF.Copy), reads=[PK[5]], writes=["hst%d" % hi])
                            b.dma("sp", lambda q, hi=hi, ti=ti: q.dma_start(out=HTOK[ti * 128:(ti + 1) * 128, g4 * 512:(g4 + 1) * 512], in_=hst[hi][:]),
                                  reads=["hst%d" % hi], writes=["HTOK"])
                    if pend["tr"] is not None:
                        pend["tr"]()
                        pend["tr"] = None
                    if c % 4 == 3:
                        pend["tr"] = trgroup
                        if c == KC - 1:
                            pend["tr"]()
                            pend["tr"] = None

            def P2post(tbi):
                t0, tn, j = TB256[tbi]
                X = xz[tbi % 2]
                kx = "xz%d" % (tbi % 2)
                X1 = x1[tbi % 2]
                k1 = "x1%d" % (tbi % 2)
                if final:
                    if j == 0:
                        b.dma("sp", lambda q: q.dma_start(out=dstv[:, :, t0:t0 + 256], in_=X1[:]), reads=[k1], writes=["outT"])
                else:
                    b.dma("sp", lambda q: q.dma_start(out=dstv[:, :, t0:t0 + 256], in_=X1[:]), reads=[k1], writes=[("xt", id(XTn))])
                if with_router:
                    for tt in range(2):
                        ti = tbi * 2 + tt
                        for k in range(KC):
                            b.op("pe", lambda e, k=k, tt=tt: e.matmul(PS[4][:, 0:36], lhsT=X[:, k, tt * 128:(tt + 1) * 128], rhs=rw[:, k, :],
                                                                       start=(k == 0), stop=(k == KC - 1)), reads=[kx, "rw"], writes=[PK[4]])
                        b.op("dve", lambda e, ti=ti: e.tensor_tensor(out=lg[:, ti, :], in0=PS[4][:, 0:36], in1=vcol(li, "rb", 0, 36), op=ALU.add),
                             reads=[PK[4], "vec%d" % li], writes=["lg"])

            nbk = len(TB256)
            loadX(0)
            for c in range(KC):
                P1chunk(0, c)
            for tbi in range(nbk):
                if tbi + 1 < nbk:
                    loadX(tbi + 1)
                P2pre(tbi)
                for c in range(KC):
                    if tbi + 1 < nbk:
                        P1chunk(tbi + 1, c)
                    P2chunk(tbi, c)
                P2post(tbi)
        b.barrier()

    def wo_ysrc(Wo, gidx, bias_col=None, li=None):
        def mk(S):
            wo = b.sb(S, "wo", [128, KC, D], BF16)
            src = Wo.rearrange("(c p) n -> p c n", p=128)
            for hlf in range(4):
                b.dma("pool", lambda q, hlf=hlf: q.dma_start(out=wo[:, :, hlf * 512:(hlf + 1) * 512], in_=src[:, :, hlf * 512:(hlf + 1) * 512]), writes=["wo"])
            ob = [b.sb(S, "ob", [128, KC, 256], BF16) for _ in range(2)]
            ysb = [b.sb(S, "ysb", [128, 256], F32) for _ in range(2)]
            osrc = OT.rearrange("(c p) t -> p c t", p=128)
            st = {"n": 0}

            def zfn(tbi, t0, j, c, X, kx):
                O = ob[tbi % 2]
                ko = "ob%d" % (tbi % 2)
                if c == 0:
                    b.dma("sp", lambda q: q.dma_start(out=O[:], in_=osrc[:, :, t0:t0 + 256]), reads=[("dstT", id(OT))], writes=[ko])
                pi = st["n"] % 2
                st["n"] += 1
                for k in range(KC):
                    b.op("pe", lambda e, k=k: e.matmul(PS[pi][:, 0:256], lhsT=wo[:, k, c * 128:(c + 1) * 128], rhs=O[:, k, :], start=(k == 0), stop=(k == KC - 1)),
                         reads=["wo", ko], writes=[PK[pi]])
                if bias_col is None:
                    b.op("dve", lambda e: e.scalar_tensor_tensor(out=X[:, c, :], in0=PS[pi][:, 0:256], scalar=der[:, gidx, c, j:j + 1], in1=X[:, c, :],
                                                                  op0=ALU.mult, op1=ALU.add), reads=[PK[pi], kx, "der"], writes=[kx])
                else:
                    Y = ysb[pi]
                    b.op("act", lambda e: e.activation(out=Y[:], in_=PS[pi][:, 0:256], func=AF.Identity, bias=vcol(li, bias_col, c)),
                         reads=[PK[pi], "vec%d" % li], writes=["ysb%d" % pi])
                    b.op("dve", lambda e: e.scalar_tensor_tensor(out=X[:, c, :], in0=Y[:], scalar=der[:, gidx, c, j:j + 1], in1=X[:, c, :],
                                                                  op0=ALU.mult, op1=ALU.add), reads=["ysb%d" % pi, kx, "der"], writes=[kx])
            return zfn
        return mk


    def phase_moe_route(li):
        with ExitStack() as S:
            t18 = lambda n: b.sb(S, n, [128, NT], F32)
            gmax, gs, gp, m1, m2, rr, den = [t18(n) for n in ["gmax", "gs", "gp", "m1", "m2", "rr", "den"]]
            d4 = b.sb(S, "d4", [128, NT, 4], F32)
            og = b.sb(S, "og", [128, NT, 4], F32)
            pen = b.sb(S, "pen", [128, NT, 4], F32)
            lm = b.sb(S, "lm", [128, NT, 32], F32)
            lm2 = b.sb(S, "lm2", [128, NT, 32], F32)
            o1 = b.sb(S, "o1", [128, NT, 32], F32)
            o2 = b.sb(S, "o2", [128, NT, 32], F32)
            Mb = b.sb(S, "Mb", [128, NT, 32], BF16)
            rk = b.sb(S, "rk", [128, NT, 32], F32)
            pr = b.sb(S, "pr", [128, NT, 32], F32)
            cnt = b.sb(S, "cnt", [128, 32], F32)
            nb_ = b.sb(S, "nb", [128, 32], F32)
            pe_ = b.sb(S, "pe", [128, 32], F32)
            pst = b.sb(S, "pst", [128, 32], F32)
            tm = b.sb(S, "tm", [128, 32], F32)
            be = b.sb(S, "be", [128, 64], F32)
            slf = b.sb(S, "slf", [128, 2, NT], F32)
            V = lambda e, fn, r, w: b.op(e, fn, reads=r, writes=w)
            V("dve", lambda e: e.tensor_reduce(out=gmax[:], in_=lg[:, :, 0:4], axis=AX.X, op=ALU.max), ["lg"], ["gmax"])
            for g in range(4):
                V("dve", lambda e, g=g: e.tensor_tensor(out=og[:, :, g], in0=lg[:, :, g], in1=gmax[:], op=ALU.is_equal), ["lg", "gmax"], ["og"])
                V("dve", lambda e, g=g: e.tensor_tensor(out=d4[:, :, g], in0=lg[:, :, g], in1=gmax[:], op=ALU.subtract), ["lg", "gmax"], ["d4"])
            V("act", lambda e: e.activation(out=d4[:], in_=d4[:], func=AF.Exp), ["d4"], ["d4"])
            V("dve", lambda e: e.tensor_reduce(out=gs[:], in_=d4[:], axis=AX.X, op=ALU.add), ["d4"], ["gs"])
            V("dve", lambda e: e.reciprocal(out=gp[:], in_=gs[:]), ["gs"], ["gp"])
            V("dve", lambda e: e.tensor_scalar(out=pen[:], in0=og[:], scalar1=1e30, scalar2=-1e30, op0=ALU.mult, op1=ALU.add), ["og"], ["pen"])
            for ex in range(32):
                V("dve", lambda e, ex=ex: e.tensor_tensor(out=lm[:, :, ex], in0=lg[:, :, 4 + ex], in1=pen[:, :, ex // 8], op=ALU.add), ["lg", "pen"], ["lm"])
            V("dve", lambda e: e.tensor_reduce(out=m1[:], in_=lm[:], axis=AX.X, op=ALU.max), ["lm"], ["m1"])
            for ex in range(32):
                V("dve", lambda e, ex=ex: e.tensor_tensor(out=o1[:, :, ex], in0=lm[:, :, ex], in1=m1[:], op=ALU.is_equal), ["lm", "m1"], ["o1"])
            V("dve", lambda e: e.scalar_tensor_tensor(out=lm2[:], in0=o1[:], scalar=-1e30, in1=lm[:], op0=ALU.mult, op1=ALU.add), ["o1", "lm"], ["lm2"])
            V("dve", lambda e: e.tensor_reduce(out=m2[:], in_=lm2[:], axis=AX.X, op=ALU.max), ["lm2"], ["m2"])
            for ex in range(32):
                V("dve", lambda e, ex=ex: e.tensor_tensor(out=o2[:, :, ex], in0=lm2[:, :, ex], in1=m2[:], op=ALU.is_equal), ["lm2", "m2"], ["o2"])
            V("dve", lambda e: e.tensor_tensor(out=rr[:], in0=m2[:], in1=m1[:], op=ALU.subtract), ["m1", "m2"], ["rr"])
            V("act", lambda e: e.activation(out=rr[:], in_=rr[:], func=AF.Exp), ["rr"], ["rr"])
            V("dve", lambda e: e.tensor_scalar(out=den[:], in0=rr[:], scalar1=1.0, scalar2=None, op0=ALU.add), ["rr"], ["den"])
            V("dve", lambda e: e.reciprocal(out=den[:], in_=den[:]), ["den"], ["den"])
            V("dve", lambda e: e.tensor_tensor(out=gate[:, 0, :], in0=gp[:], in1=den[:], op=ALU.mult), ["gp", "den"], ["gate"])
            V("dve", lambda e: e.tensor_tensor(out=gate[:, 1, :], in0=gate[:, 0, :], in1=rr[:], op=ALU.mult), ["gate", "rr"], ["gate"])
            V("dve", lambda e: e.tensor_tensor(out=Mb[:], in0=o1[:], in1=o2[:], op=ALU.add), ["o1", "o2"], ["Mb"])
            for i in range(NT):
                pi = i % 2
                V("pe", lambda e, i=i, pi=pi: e.matmul(PS[pi][:, 0:32], lhsT=utb[:], rhs=Mb[:, i, :], start=True, stop=(i == 0)), ["utb", "Mb"], [PK[pi]])
                for jx in range(i):
                    V("pe", lambda e, i=i, jx=jx, pi=pi: e.matmul(PS[pi][:, 0:32], lhsT=onesb[:], rhs=Mb[:, jx, :], start=False, stop=(jx == i - 1)),
                      ["onesb", "Mb"], [PK[pi]])
                V("dve", lambda e, i=i, pi=pi: e.tensor_copy(out=rk[:, i, :], in_=PS[pi][:, 0:32]), [PK[pi]], ["rk"])
            for jx in range(NT):
                V("pe", lambda e, jx=jx: e.matmul(PS[2][:, 0:32], lhsT=onesb[:], rhs=Mb[:, jx, :], start=(jx == 0), stop=(jx == NT - 1)), ["onesb", "Mb"], [PK[2]])
            V("dve", lambda e: e.tensor_copy(out=cnt[:], in_=PS[2][:, 0:32]), [PK[2]], ["cnt"])
            V("dve", lambda e: e.tensor_single_scalar(out=nb_[:], in_=cnt[:], scalar=0.0, op=ALU.is_gt), ["cnt"], ["nb"])
            for jx in range(1, 10):
                V("dve", lambda e, jx=jx: e.tensor_single_scalar(out=tm[:], in_=cnt[:], scalar=float(jx * BLK), op=ALU.is_gt), ["cnt"], ["tm"])
                V("dve", lambda e: e.tensor_tensor(out=nb_[:], in0=nb_[:], in1=tm[:], op=ALU.add), ["nb", "tm"], ["nb"])
            V("dve", lambda e: e.tensor_copy(out=pe_[:, 0:1], in_=nb_[:, 0:1]), ["nb"], ["pe"])
            for ex in range(1, 32):
                V("dve", lambda e, ex=ex: e.tensor_tensor(out=pe_[:, ex:ex + 1], in0=pe_[:, ex - 1:ex], in1=nb_[:, ex:ex + 1], op=ALU.add), ["pe", "nb"], ["pe"])
            V("dve", lambda e: e.tensor_tensor(out=pst[:], in0=pe_[:], in1=nb_[:], op=ALU.subtract), ["pe", "nb"], ["pst"])
            V("dve", lambda e: e.tensor_scalar(out=pst[:], in0=pst[:], scalar1=float(BLK), scalar2=None, op0=ALU.mult), ["pst"], ["pst"])
            for i in range(NT):
                V("dve", lambda e, i=i: e.tensor_tensor(out=rk[:, i, :], in0=rk[:, i, :], in1=pst[:], op=ALU.add), ["rk", "pst"], ["rk"])
            for k, ok in enumerate([o1, o2]):
                V("dve", lambda e, ok=ok: e.tensor_tensor(out=pr[:], in0=ok[:], in1=rk[:], op=ALU.mult), ["o1", "o2", "rk"], ["pr"])
                V("dve", lambda e, k=k: e.tensor_reduce(out=slf[:, k, :], in_=pr[:], axis=AX.X, op=ALU.add), ["pr"], ["slf"])
            V("dve", lambda e: e.tensor_copy(out=slot_i[:], in_=slf[:]), ["slf"], ["slot_i"])
            V("dve", lambda e: e.memset(be[:], 0.0), [], ["be"])
            for ex in range(32):
                V("dve", lambda e, ex=ex: e.scalar_tensor_tensor(out=be[:], in0=cst2[:, 0:64], scalar=pe_[:, ex:ex + 1], in1=be[:], op0=ALU.is_ge, op1=ALU.add),
                  ["cst2", "pe", "be"], ["be"])
            V("dve", lambda e: e.tensor_scalar(out=be[:], in0=be[:], scalar1=128.0, scalar2=cst2[:, 64:65], op0=ALU.mult, op1=ALU.add), ["be", "cst2"], ["be"])
            V("dve", lambda e: e.tensor_copy(out=widx[:], in_=be[:, 0:NBLK]), ["be"], ["widx"])
            hs = [b.sb(S, "hs", [128, D], BF16) for _ in range(3)]
            for i in range(NT):
                H = hs[i % 3]
                kh_ = "hs%d" % (i % 3)
                b.dma("sp", lambda q, H=H, i=i: q.dma_start(out=H[:], in_=HTOK[i * 128:(i + 1) * 128, :]), reads=["HTOK"], writes=[kh_])
                for k in range(2):
                    b.dma("pool", lambda q, H=H, i=i, k=k: q.indirect_dma_start(out=XS, out_offset=bass.IndirectOffsetOnAxis(ap=slot_i[:, k, i:i + 1], axis=0),
                                                                              in_=H[:], in_offset=None), reads=[kh_, "slot_i"], writes=["XS"])
        b.barrier()

    BCREG = nc.gpsimd.to_reg(32 * 128 - 1)

    def phase_moe_blocks(li):
        with ExitStack() as S:
            xsb = b.sb(S, "xsb", [128, 2, D], BF16)
            xsT = [b.sb(S, "xsT", [128, KC, 256], BF16) for _ in range(2)]
            w13b = [[b.sb(S, "w13b", [128, 4096], BF16) for _ in range(4)] for _ in range(2)]
            w2b = [[b.sb(S, "w2b", [128, 2048], BF16) for _ in range(4)] for _ in range(2)]
            wst = [b.sb(S, "wst", [128, 4096], F32) for _ in range(2)]
            wst2 = [b.sb(S, "wst2", [128, 2048], F32) for _ in range(2)]
            sg = [b.sb(S, "sg", [128, 256], F32) for _ in range(2)]
            gT = [b.sb(S, "gT", [128, 4, 256], BF16) for _ in range(2)]
            yst = [b.sb(S, "yst", [128, D], BF16) for _ in range(2)]
            xsv = XS.rearrange("(j s p) d -> j p s d", s=2, p=128)
            cnt = {"w": 0, "w2": 0, "o": 0, "y": 0}

            def load13(j, m):
                r = j % 2
                wi = cnt["w"] % 2
                cnt["w"] += 1
                b.dma("pool", lambda q: q.indirect_dma_start(out=wst[wi][:], out_offset=None, in_=w13_in[li][m],
                                                             in_offset=bass.IndirectOffsetOnAxis(ap=widx[:, j:j + 1], axis=0), bounds_check=BCREG, oob_is_err=False),
                      reads=["widx"], writes=["wst%d" % wi])
                b.op("dve", lambda e: e.tensor_copy(out=w13b[r][m][:], in_=wst[wi][:]), reads=["wst%d" % wi], writes=["w13b%d%d" % (r, m)])

            def load2(j, m):
                r = j % 2
                wi = cnt["w2"] % 2
                cnt["w2"] += 1
                b.dma("pool", lambda q: q.indirect_dma_start(out=wst2[wi][:], out_offset=None, in_=w2_in[li][m],
                                                             in_offset=bass.IndirectOffsetOnAxis(ap=widx[:, j:j + 1], axis=0), bounds_check=BCREG, oob_is_err=False),
                      reads=["widx"], writes=["wst2%d" % wi])
                b.op("act", lambda e: e.activation(out=w2b[r][m][:], in_=wst2[wi][:], func=AF.Copy), reads=["wst2%d" % wi], writes=["w2b%d%d" % (r, m)])

            def loadxs(j):
                b.dma("sp", lambda q: q.dma_start(out=xsb[:], in_=xsv[j]), reads=["XS"], writes=["xsb"])

            loadxs(0)
            for m in range(4):
                load13(0, m)
                load2(0, m)
            for j in range(NBLK):
                r = j % 2
                for s2 in range(2):
                    for hh in range(2):
                        pi = (s2 * 2 + hh) % 2
                        for cc in range(8):
                            c = hh * 8 + cc
                            b.op("pe", lambda e, s2=s2, c=c, cc=cc, pi=pi: e.transpose(PS[pi][:].bitcast(BF16)[:, cc * 128:(cc + 1) * 128],
                                                                                       xsb[:, s2, c * 128:(c + 1) * 128], identb[:]),
                                 reads=["xsb", "identb"], writes=[PK[pi]])
                        b.op("act", lambda e, r=r, s2=s2, hh=hh, pi=pi: e.activation(out=xsT[r][:, hh * 8:(hh + 1) * 8, s2 * 128:(s2 + 1) * 128],
                                                                                     in_=PS[pi][:].bitcast(BF16).rearrange("p (c t) -> p c t", c=8), func=AF.Copy),
                             reads=[PK[pi]], writes=["xsT%d" % r])
                if j + 1 < NBLK:
                    loadxs(j + 1)
                for m in range(4):
                    pa = 2 + 2 * (m % 2)
                    for half in range(2):
                        for c in range(KC):
                            b.op("pe", lambda e, r=r, m=m, half=half, c=c, pa=pa: e.matmul(PS[pa + half][:, 0:256],
                                                                                          lhsT=w13b[r][m][:, c * 256 + half * 128:c * 256 + half * 128 + 128],
                                                                                          rhs=xsT[r][:, c, :], start=(c == 0), stop=(c == KC - 1)),
                                 reads=["w13b%d%d" % (r, m), "xsT%d" % r], writes=[PK[pa + half]])
                    si = m % 2
                    b.op("act", lambda e, si=si, pa=pa: e.activation(out=sg[si][:], in_=PS[pa][:, 0:256], func=AF.Silu), reads=[PK[pa]], writes=["sg%d" % si])
                    b.op("dve", lambda e, si=si, pa=pa, r=r, m=m: e.tensor_tensor(out=gT[r][:, m, :], in0=PS[pa + 1][:, 0:256], in1=sg[si][:], op=ALU.mult),
                         reads=[PK[pa + 1], "sg%d" % si], writes=["gT%d" % r])
                    if j + 1 < NBLK:
                        load13(j + 1, m)
                        load2(j + 1, m)
                for s2 in range(2):
                    Y = yst[cnt["y"] % 2]
                    ky = "yst%d" % (cnt["y"] % 2)
                    cnt["y"] += 1
                    for nbk in range(4):
                        po = 6 + cnt["o"] % 2
                        cnt["o"] += 1
                        for c4 in range(4):
                            b.op("pe", lambda e, r=r, s2=s2, nbk=nbk, c4=c4, po=po: e.matmul(PS[po][:], lhsT=gT[r][:, c4, s2 * 128:(s2 + 1) * 128],
                                                                                            rhs=w2b[r][nbk][:, c4 * 512:(c4 + 1) * 512], start=(c4 == 0), stop=(c4 == 3)),
                                 reads=["gT%d" % r, "w2b%d%d" % (r, nbk)], writes=[PK[po]])
                        b.op("act", lambda e, Y=Y, nbk=nbk, po=po: e.activation(out=Y[:, nbk * 512:(nbk + 1) * 512], in_=PS[po][:], func=AF.Copy), reads=[PK[po]], writes=[ky])
                    b.dma("sp", lambda q, Y=Y, j=j, s2=s2: q.dma_start(out=YS[j * 256 + s2 * 128:j * 256 + (s2 + 1) * 128, :], in_=Y[:]), reads=[ky], writes=["YS"])
        b.barrier()

    def moe_ysrc(gidx):
        def mk(S):
            yg = [[b.sb(S, "yg", [128, D], BF16) for _ in range(2)] for _ in range(2)]
            ff = [b.sb(S, "ff", [128, D], F32) for _ in range(2)]
            st = {"n": 0}

            def zfn(tbi, t0, j, c, X, kx):
                if c == 0:
                    for tt in range(2):
                        ti = tbi * 2 + tt
                        for k in range(2):
                            b.dma("pool", lambda q, tt=tt, k=k, ti=ti: q.indirect_dma_start(out=yg[tt][k][:], out_offset=None, in_=YS,
                                                                                          in_offset=bass.IndirectOffsetOnAxis(ap=slot_i[:, k, ti:ti + 1], axis=0)),
                                  reads=["YS", "slot_i"], writes=["yg%d%d" % (tt, k)])
                        b.op("act", lambda e, tt=tt, ti=ti: e.activation(out=ff[tt][:], in_=yg[tt][0][:], func=AF.Copy, scale=gate[:, 0, ti:ti + 1]),
                             reads=["yg%d0" % tt, "gate"], writes=["ff%d" % tt])
                        b.op("dve", lambda e, tt=tt, ti=ti: e.scalar_tensor_tensor(out=ff[tt][:], in0=yg[tt][1][:], scalar=gate[:, 1, ti:ti + 1], in1=ff[tt][:],
                                                                                  op0=ALU.mult, op1=ALU.add), reads=["yg%d1" % tt, "gate", "ff%d" % tt], writes=["ff%d" % tt])
                pi = st["n"] % 2
                st["n"] += 1
                for tt in range(2):
                    b.op("pe", lambda e, tt=tt: e.transpose(PS[pi][:, tt * 128:(tt + 1) * 128], ff[tt][:, c * 128:(c + 1) * 128], identf),
                         reads=["ff%d" % tt, "cst"], writes=[PK[pi]])
                b.op("dve", lambda e: e.scalar_tensor_tensor(out=X[:, c, :], in0=PS[pi][:, 0:256], scalar=der[:, gidx, c, j:j + 1], in1=X[:, c, :],
                                                              op0=ALU.mult, op1=ALU.add), reads=[PK[pi], kx, "der"], writes=[kx])
            return zfn
        return mk


    esink = b.sb(G, "esink", [128, 16], F32)

    def swa_qblocks(h):
        res = []
        ctxc = [(2048, "none", None), (2176, "none", None)]
        for bq in range(4):
            q0 = bq * 512
            ch = []
            for jx in range(6):
                k0 = q0 - 128 + jx * 128
                if 0 <= k0 < L:
                    ch.append((k0, "mask", jx))
            res.append((q0, 512, ch + ctxc))
        res.append((2048, 256, ctxc))
        return res

    def phase_attn_swa():
        b.op("act", lambda e: e.activation(out=esink[:], in_=vcol(2, "sink", 0, 16), func=AF.Exp), reads=["vec2"], writes=["esink"])
        with ExitStack() as S:
            heads = [(h * 128, (h // 4) * 128, (h // 4) * 128, h) for h in range(16)]
            attn_head_loop(S, heads, 128, swa_qblocks, simple_finish(S), extra_den=lambda h: esink[:, h:h + 1])
        b.barrier()

    LAM_INIT = 0.8 - 0.6 * math.exp(-0.3 * 3)
    nlam = b.sb(G, "nlam", [128, 1], F32)
    subgl = b.sb(G, "subgl", [128, 2], F32)
    lamt = b.sb(G, "lamt", [128, 2], F32)

    def diff_qblocks(tag):
        allk = [(i * 128, "none", None) for i in range(NT)]
        res = [(bq * 512, 512, allk) for bq in range(4)]
        res.append((2048, 256, [(2048, "none", None), (2176, "none", None)]))
        return res

    def phase_attn_diff():
        R3 = ["vec3"]
        b.op("dve", lambda e: e.tensor_tensor(out=lamt[:, 0:1], in0=vcol(3, "lam", 0), in1=vcol(3, "lam", 1), op=ALU.mult), reads=R3, writes=["lamt"])
        b.op("dve", lambda e: e.tensor_tensor(out=lamt[:, 1:2], in0=vcol(3, "lam", 2), in1=vcol(3, "lam", 3), op=ALU.mult), reads=R3, writes=["lamt"])
        b.op("pe", lambda e: e.matmul(PS[5][:, 0:2], lhsT=cst[:, 1, :], rhs=lamt[:], start=True, stop=True), reads=["cst", "lamt"], writes=[PK[5]])
        b.op("act", lambda e: e.activation(out=lamt[:], in_=PS[5][:, 0:2], func=AF.Exp), reads=[PK[5]], writes=["lamt"])
        b.op("dve", lambda e: e.tensor_tensor(out=nlam[:], in0=lamt[:, 1:2], in1=lamt[:, 0:1], op=ALU.subtract), reads=["lamt"], writes=["nlam"])
        b.op("dve", lambda e: e.tensor_scalar(out=nlam[:], in0=nlam[:], scalar1=-LAM_INIT, scalar2=None, op0=ALU.add), reads=["nlam"], writes=["nlam"])
        b.op("dve", lambda e: e.tensor_scalar(out=subgl[:], in0=vcol(3, "subg", 0, 2), scalar1=1.0 - LAM_INIT, scalar2=None, op0=ALU.mult), reads=R3, writes=["subgl"])
        with ExitStack() as S:
            o0 = b.sb(S, "o0", [128, 2, T], F32)
            od = [b.sb(S, "od", [128, 2, 512], F32) for _ in range(2)]
            osq = [b.sb(S, "osq", [128, 512], BF16) for _ in range(2)]
            rs = [b.sb(S, "rs", [128, 512], F32) for _ in range(2)]
            ostg = [b.sb(S, "dostg", [128, 2, T], BF16) for _ in range(2)]
            st = {"n": 0}

            def fin(hn, tag, q0, nq, rec, krec, ob, rb):
                h, m = tag
                if m == 0:
                    for dv in range(2):
                        b.op("dve", lambda e, dv=dv: e.tensor_tensor(out=o0[:, dv, q0:q0 + nq], in0=PS[ob[dv]][:, 0:nq], in1=rec[:, 0:nq], op=ALU.mult),
                             reads=[PK[ob[dv]], krec], writes=["o0"])
                    return
                i = st["n"] % 2
                st["n"] += 1
                O = od[i]
                ko = "od%d" % i
                og = ostg[h % 2]
                kg = "dostg%d" % (h % 2)
                for dv in range(2):
                    b.op("dve", lambda e, dv=dv: e.tensor_tensor(out=O[:, dv, 0:nq], in0=PS[ob[dv]][:, 0:nq], in1=rec[:, 0:nq], op=ALU.mult),
                         reads=[PK[ob[dv]], krec], writes=[ko])
                    b.op("dve", lambda e, dv=dv: e.scalar_tensor_tensor(out=O[:, dv, 0:nq], in0=O[:, dv, 0:nq], scalar=nlam[:, 0:1], in1=o0[:, dv, q0:q0 + nq],
                                                                        op0=ALU.mult, op1=ALU.add), reads=[ko, "nlam", "o0"], writes=[ko])
                    b.op("act", lambda e, dv=dv: e.activation(out=osq[dv][:, 0:nq], in_=O[:, dv, 0:nq], func=AF.Square), reads=[ko], writes=["osq%d" % dv])
                    b.op("pe", lambda e, dv=dv: e.matmul(PS[rb][:, 0:nq], lhsT=onesb[:], rhs=osq[dv][:, 0:nq], start=(dv == 0), stop=(dv == 1)),
                         reads=["osq%d" % dv, "onesb"], writes=[PK[rb]])
                R_ = rs[i]
                kr = "rs%d" % i
                b.op("dve", lambda e: e.tensor_scalar(out=R_[:, 0:nq], in0=PS[rb][:, 0:nq], scalar1=1.0 / 256.0, scalar2=EPS, op0=ALU.mult, op1=ALU.add),
                     reads=[PK[rb]], writes=[kr])
                b.op("act", lambda e: e.activation(out=R_[:, 0:nq], in_=R_[:, 0:nq], func=AF.Sqrt), reads=[kr], writes=[kr])
                b.op("dve", lambda e: e.reciprocal(out=R_[:, 0:nq], in_=R_[:, 0:nq]), reads=[kr], writes=[kr])
                for dv in range(2):
                    b.op("dve", lambda e, dv=dv: e.tensor_tensor(out=O[:, dv, 0:nq], in0=O[:, dv, 0:nq], in1=R_[:, 0:nq], op=ALU.mult), reads=[ko, kr], writes=[ko])
                    b.op("act", lambda e, dv=dv: e.activation(out=og[:, dv, q0:q0 + nq], in_=O[:, dv, 0:nq], func=AF.Copy, scale=subgl[:, dv:dv + 1]),
                         reads=[ko, "subgl"], writes=[kg])
                if q0 + nq == T:
                    for dv in range(2):
                        b.dma("sp", lambda q, dv=dv: q.dma_start(out=OT[h * 256 + dv * 128:h * 256 + (dv + 1) * 128, :], in_=og[:, dv, :]),
                              reads=[kg], writes=[("dstT", id(OT))])

            heads = [((2 * h + m) * 128, (2 * h + m) * 128, h * 256, (h, m)) for h in range(8) for m in range(2)]
            attn_head_loop(S, heads, 256, diff_qblocks, fin)
        b.barrier()

    GL = 15 + L + 15
    GW = GL + 15 + C + 15

    def phase_conv_a(XT):
        with ExitStack() as S:
            uT = build_u(S, XT)
            wq = [b.sb(S, "wgl", [128, KC, 512], BF16) for _ in range(2)]
            Gb = [b.sb(S, "Gb", [128, GW], F32) for _ in range(2)]
            acc = [b.sb(S, "cacc", [128, T], F32) for _ in range(2)]
            sig = [b.sb(S, "sig", [128, 512], F32) for _ in range(2)]
            for i in range(2):
                b.op("pool", lambda e, i=i: e.memset(Gb[i][:], 0.0), writes=["Gb%d" % i])
            src = win1.rearrange("(c p) n -> p c n", p=128)
            cnt = 0
            for pc in range(8):
                wb = wq[pc % 2]
                kw = "wgl%d" % (pc % 2)
                b.dma("pool", lambda q, wb=wb, pc=pc: q.dma_start(out=wb[:], in_=src[:, :, pc * 512:(pc + 1) * 512]), writes=[kw])
                for f2 in range(2):
                    fc = pc * 2 + f2
                    Gt = Gb[fc % 2]
                    kg = "Gb%d" % (fc % 2)
                    A = acc[fc % 2]
                    ka = "cacc%d" % (fc % 2)
                    for (t0, tn, j) in TB512:
                        pi = cnt % 2
                        cnt += 1
                        for half in range(2):
                            for k in range(KC):
                                b.op("pe", lambda e, k=k, half=half, pi=pi, t0=t0, tn=tn, wb=wb, f2=f2: e.matmul(
                                    PS[2 * pi + half][:, 0:tn], lhsT=wb[:, k, f2 * 256 + half * 128:f2 * 256 + half * 128 + 128],
                                    rhs=uT[:, k, t0:t0 + tn], start=(k == 0), stop=(k == KC - 1)), reads=[kw, "uT"], writes=[PK[2 * pi + half]])
                        b.op("act", lambda e, pi=pi, tn=tn, fc=fc: e.activation(out=sig[pi][:, 0:tn], in_=PS[2 * pi + 1][:, 0:tn], func=AF.Sigmoid,
                                                                               bias=vcol(1, "b_in", 16 + fc)), reads=[PK[2 * pi + 1], "vec1"], writes=["sig%d" % pi])
                        g0 = 15 + t0 if j == 0 else GL + 15
                        b.op("dve", lambda e, pi=pi, tn=tn, fc=fc, g0=g0, Gt=Gt: e.scalar_tensor_tensor(
                            out=Gt[:, g0:g0 + tn], in0=PS[2 * pi][:, 0:tn], scalar=vcol(1, "b_in", fc), in1=sig[pi][:, 0:tn], op0=ALU.add, op1=ALU.mult),
                            reads=[PK[2 * pi], "sig%d" % pi, "vec1"], writes=[kg])
                    for (a0, an, gb) in [(0, L, 0), (L, C, GL)]:
                        b.op("dve", lambda e, A=A, Gt=Gt, a0=a0, an=an, gb=gb, fc=fc: e.tensor_scalar(
                            out=A[:, a0:a0 + an], in0=Gt[:, gb:gb + an], scalar1=vcol(1, "dw", fc), scalar2=vcol(1, "dw_b", fc), op0=ALU.mult, op1=ALU.add),
                            reads=[kg, "vec1"], writes=[ka])
                        for k in range(1, 31):
                            b.op("dve", lambda e, A=A, Gt=Gt, a0=a0, an=an, gb=gb, fc=fc, k=k: e.scalar_tensor_tensor(
                                out=A[:, a0:a0 + an], in0=Gt[:, gb + k:gb + k + an], scalar=vcol(1, "dw", k * 16 + fc), in1=A[:, a0:a0 + an],
                                op0=ALU.mult, op1=ALU.add), reads=[kg, ka, "vec1"], writes=[ka])
                    b.dma("sp", lambda q, A=A, fc=fc: q.dma_start(out=CT[fc * 128:(fc + 1) * 128, :], in_=A[:]), reads=[ka], writes=["CT"])
        b.barrier()

    def phase_conv_b():
        with ExitStack() as S:
            xz = [b.sb(S, "cz", [128, KC, 256], F32) for _ in range(2)]
            ob = [b.sb(S, "cob", [128, KC, 256], BF16) for _ in range(2)]
            zb = [b.sb(S, "czb", [128, 256], BF16) for _ in range(3)]
            zq = [b.sb(S, "czq", [128, 256], BF16) for _ in range(3)]
            mean = b.sb(S, "cmean", [128, 256], F32)
            rstd = b.sb(S, "crstd", [128, 256], F32)
            var = b.sb(S, "cvar", [128, 256], F32)
            src = CT.rearrange("(c p) t -> p c t", p=128)
            dst = OT.rearrange("(c p) t -> p c t", p=128)
            cz = 0
            for tbi, (t0, tn, j) in enumerate(TB256):
                X = xz[tbi % 2]
                kx = "cz%d" % (tbi % 2)
                O = ob[tbi % 2]
                ko = "cob%d" % (tbi % 2)
                b.dma("sp", lambda q, X=X, t0=t0: q.dma_start(out=X[:], in_=src[:, :, t0:t0 + 256]), reads=["CT"], writes=[kx])
                for c in range(KC):
                    zi = cz % 3
                    cz += 1
                    b.op("act", lambda e, zi=zi, X=X, c=c: e.activation(out=zb[zi][:], in_=X[:, c, :], func=AF.Copy), reads=[kx], writes=["czb%d" % zi])
                    b.op("act", lambda e, zi=zi, X=X, c=c: e.activation(out=zq[zi][:], in_=X[:, c, :], func=AF.Square), reads=[kx], writes=["czq%d" % zi])
                    b.op("pe", lambda e, zi=zi, c=c: e.matmul(PS[2][:, 0:256], lhsT=onesb[:], rhs=zb[zi][:], start=(c == 0), stop=(c == KC - 1)),
                         reads=["czb%d" % zi, "onesb"], writes=[PK[2]])
                    b.op("pe", lambda e, zi=zi, c=c: e.matmul(PS[3][:, 0:256], lhsT=onesb[:], rhs=zq[zi][:], start=(c == 0), stop=(c == KC - 1)),
                         reads=["czq%d" % zi, "onesb"], writes=[PK[3]])
                b.op("dve", lambda e: e.tensor_scalar(out=mean[:], in0=PS[2][:, 0:256], scalar1=1.0 / D, scalar2=None, op0=ALU.mult), reads=[PK[2]], writes=["cmean"])
                b.op("dve", lambda e: e.tensor_tensor(out=var[:], in0=mean[:], in1=mean[:], op=ALU.mult), reads=["cmean"], writes=["cvar"])
                b.op("dve", lambda e: e.scalar_tensor_tensor(out=var[:], in0=PS[3][:, 0:256], scalar=1.0 / D, in1=var[:], op0=ALU.mult, op1=ALU.subtract),
                     reads=[PK[3], "cvar"], writes=["cvar"])
                b.op("dve", lambda e: e.tensor_scalar(out=var[:], in0=var[:], scalar1=EPS, scalar2=None, op0=ALU.add), reads=["cvar"], writes=["cvar"])
                b.op("act", lambda e: e.activation(out=var[:], in_=var[:], func=AF.Sqrt), reads=["cvar"], writes=["cvar"])
                b.op("dve", lambda e: e.reciprocal(out=rstd[:], in_=var[:]), reads=["cvar"], writes=["crstd"])
                for c in range(KC):
                    b.op("dve", lambda e, X=X, c=c: e.tensor_tensor(out=X[:, c, :], in0=X[:, c, :], in1=mean[:], op=ALU.subtract), reads=[kx, "cmean"], writes=[kx])
                    b.op("dve", lambda e, X=X, c=c: e.tensor_tensor(out=X[:, c, :], in0=X[:, c, :], in1=rstd[:], op=ALU.mult), reads=[kx, "crstd"], writes=[kx])
                    b.op("act", lambda e, X=X, O=O, c=c: e.activation(out=X[:, c, :], in_=X[:, c, :], func=AF.Identity,
                                                                      scale=vcol(1, "cln_g", c), bias=vcol(1, "cln_b", c)), reads=[kx, "vec1"], writes=[kx])
                    b.op("act", lambda e, X=X, O=O, c=c: e.activation(out=O[:, c, :], in_=X[:, c, :], func=AF.Silu), reads=[kx], writes=[ko])
                b.dma("sp", lambda q, O=O, t0=t0: q.dma_start(out=dst[:, :, t0:t0 + 256], in_=O[:]), reads=[ko], writes=[("dstT", id(OT))])
        b.barrier()

    phase_mod()
    stop = {"l0": 0, "l1": 1, "l2": 2, "l3": 3}.get(dbg, 3)
    Xin = xT_in
    for li in range(stop + 1):
        derive(li)
        if li == 0:
            phase_premix_attn(0, Xin, wqkv0, 16, 16, 2048, False)
            phase_attn_na()
            ln_phase(0, Xin, XA, wo_ysrc(wo0, 2), 2, "ln1_g", "ln1_b", True)
        elif li == 1:
            phase_conv_a(Xin)
            phase_conv_b()
            ln_phase(1, Xin, XA, wo_ysrc(wo1, 2, "b_out", 1), 2, "ln1_g", "ln1_b", True)
        elif li == 2:
            phase_premix_attn(2, Xin, wqkv2, 16, 4, 512, True)
            phase_attn_swa()
            ln_phase(2, Xin, XA, wo_ysrc(wo2, 2), 2, "ln1_g", "ln1_b", True)
        else:
            phase_premix_attn(3, Xin, wqkv3, 16, 16, 2048, True)
            phase_attn_diff()
            ln_phase(3, Xin, XA, wo_ysrc(wo3, 2), 2, "ln1_g", "ln1_b", True)
        phase_moe_route(li)
        phase_moe_blocks(li)
        ln_phase(li, XA, XB, moe_ysrc(5), 5, "ln2_g", "ln2_b", False, final=(li == 3))
        Xin = XB
    finish(b)
    return b


def finish(b):
    b.barrier()


def host_consts():
    ident = np.eye(128, dtype=np.float32)
    ones = np.ones((128, 128), np.float32)
    P = np.zeros((128, 128), np.float32)
    for d in range(128):
        blk, r = divmod(d, 64)
        partner = blk * 64 + (r + 32) % 64
        P[partner, d] = 1.0
    UT = np.triu(np.ones((128, 128), np.float32), 1)
    cst = np.ascontiguousarray(np.stack([ident, ones, P, UT], 1))
    t = np.arange(L)
    pos = np.stack([t // 64, t % 64], -1).astype(np.float32)
    inv = (10000.0 ** (-np.arange(32, dtype=np.float32) / 32)).astype(np.float32)
    ang = pos[:, :, None] * inv
    cos = np.cos(ang).astype(np.float32)
    sin = np.sin(ang).astype(np.float32)
    Ct = np.ones((128, T), np.float32)
    St = np.zeros((128, T), np.float32)
    for ax in range(2):
        Ct[ax * 64:ax * 64 + 32, :L] = cos[:, ax, :].T
        Ct[ax * 64 + 32:ax * 64 + 64, :L] = cos[:, ax, :].T
        St[ax * 64:ax * 64 + 32, :L] = -sin[:, ax, :].T
        St[ax * 64 + 32:ax * 64 + 64, :L] = sin[:, ax, :].T
    rope = np.ascontiguousarray(np.stack([Ct, St], 0))
    swm = np.zeros((6, 128, 512), np.float32)
    for j in range(6):
        kp = (j - 1) * 128 + np.arange(128)[:, None]
        qp = np.arange(512)[None, :]
        swm[j] = (np.abs(qp - kp) <= 128).astype(np.float32)
    cst2 = np.zeros((128, 66), np.float32)
    cst2[:, 0:64] = np.arange(64, dtype=np.float32)[None, :]
    cst2[:, 64] = np.arange(128, dtype=np.float32)
    return cst, rope, swm, cst2


def na_bias_tiles(rpb):
    rows = 32
    out = np.full((16, 20, 128, 512), NEG, np.float32)
    combos = [(0, kc, kc) for kc in range(6)] + [(1, 2 + j, 6 + j) for j in range(8)] + [(3, kc, 14 + kc - 10) for kc in range(10, 16)]
    qi = np.arange(512)
    qr_l, qc = qi // 64, qi % 64
    ki = np.arange(128)
    kr_l, kcn = ki // 64, ki % 64
    for (bq, kc, ti) in combos:
        qr = bq * 8 + qr_l
        kr = kc * 2 + kr_l
        r0 = np.clip(qr - 4, 0, rows - 8)
        okr = (kr[:, None] >= r0[None, :]) & (kr[:, None] < r0[None, :] + 8)
        cs = np.clip(qc - 8, 0, 64 - 16)
        okc = (kcn[:, None] >= cs[None, :]) & (kcn[:, None] < cs[None, :] + 16)
        dr = np.clip(kr[:, None] - qr[None, :] + 7, 0, 14)
        dc = np.clip(kcn[:, None] - qc[None, :] + 15, 0, 30)
        g = rpb[:, dr, dc]
        out[:, ti] = np.where((okr & okc)[None], g, np.float32(NEG))
    return out


def host_prepare(inp):
    cst, rope, swm, cst2 = host_consts()
    shared = {"cst": cst, "rope": rope, "swm": swm, "cst2": cst2}
    shared["nab"] = na_bias_tiles(np.asarray(inp["l0_na_rpb"], np.float32))
    kinds = [0, 1, 2, 3]
    for i in range(4):
        p = "l%d_" % i
        cols, n = vec_layout(kinds[i])
        v = np.zeros((128, n), np.float32)
        v[:, cols["mod_b"]:cols["mod_b"] + 96] = pk(inp[p + "mod_b"])
        for nm in ["ln1_g", "ln1_b", "ln2_g", "ln2_b"]:
            v[:, cols[nm]:cols[nm] + 16] = pk(inp[p + nm])
        rb = np.concatenate([inp[p + "router_g_b"], inp[p + "router_e_b"]]).astype(np.float32)
        v[:, cols["rb"]:cols["rb"] + 36] = rb[None, :]
        if i == 1:
            v[:, cols["b_in"]:cols["b_in"] + 32] = pk(inp[p + "cv_b_in"])
            dw = np.asarray(inp[p + "cv_dw"], np.float32)
            for k in range(31):
                v[:, cols["dw"] + k * 16:cols["dw"] + (k + 1) * 16] = pk(dw[k])
            v[:, cols["dw_b"]:cols["dw_b"] + 16] = pk(inp[p + "cv_dw_b"])
            v[:, cols["cln_g"]:cols["cln_g"] + 16] = pk(inp[p + "cv_ln_g"])
            v[:, cols["cln_b"]:cols["cln_b"] + 16] = pk(inp[p + "cv_ln_b"])
            v[:, cols["b_out"]:cols["b_out"] + 16] = pk(inp[p + "cv_b_out"])
        if i == 2:
            v[:, cols["sink"]:cols["sink"] + 16] = np.asarray(inp[p + "sw_sink"], np.float32)[None, :]
        if i == 3:
            v[:, cols["subg"]:cols["subg"] + 2] = pk(inp[p + "df_subln_g"])
            v[:, cols["lam"]:cols["lam"] + 4] = np.asarray(inp[p + "df_lambda"], np.float32).T
        shared["vec%d" % i] = v
        shared["modw%d" % i] = np.asarray(inp[p + "mod_w"], np.float32)
        rwm = np.concatenate([inp[p + "router_g_w"], inp[p + "router_e_w"]], 1).astype(np.float32)
        shared["rw%d" % i] = np.ascontiguousarray(rwm.reshape(KC, 128, 36).transpose(1, 0, 2))
        w13 = np.asarray(inp[p + "moe_w13"], np.float32)
        w = w13.reshape(32, KC, 128, 2, 4, 128)
        w = w.transpose(0, 2, 4, 1, 3, 5)
        for m in range(4):
            shared["w13_%d_%d" % (i, m)] = np.ascontiguousarray(w[:, :, m]).reshape(32 * 128, 4096)
        w2 = np.asarray(inp[p + "moe_w2"], np.float32)
        w = w2.reshape(32, 4, 128, 4, 512).transpose(0, 2, 3, 1, 4)
        for m in range(4):
            shared["w2_%d_%d" % (i, m)] = np.ascontiguousarray(w[:, :, m]).reshape(32 * 128, 2048)
    shared["wqkv0"] = np.asarray(inp["l0_na_w_qkv"], np.float32)
    shared["wo0"] = np.asarray(inp["l0_na_w_o"], np.float32)
    win = np.asarray(inp["l1_cv_w_in"], np.float32)
    shared["win1"] = np.ascontiguousarray(np.stack([win[:, :D].reshape(D, KC, 128), win[:, D:].reshape(D, KC, 128)], 2)).reshape(D, 4096)
    shared["wo1"] = np.asarray(inp["l1_cv_w_out"], np.float32)
    shared["wqkv2"] = np.asarray(inp["l2_sw_w_qkv"], np.float32)
    shared["wo2"] = np.asarray(inp["l2_sw_w_o"], np.float32)
    shared["wqkv3"] = np.asarray(inp["l3_df_w_qkv"], np.float32)
    shared["wo3"] = np.asarray(inp["l3_df_w_o"], np.float32)
    x = np.asarray(inp["x"], np.float32)
    ctx = np.asarray(inp["ctx"], np.float32)
    c = np.asarray(inp["c"], np.float32)
    cc = np.asarray(inp["c_ctx"], np.float32)
    maps = []
    for bi in range(8):
        m = dict(shared)
        m["xT"] = np.ascontiguousarray(np.concatenate([x[bi], ctx[bi]], 0).T)
        m["cT"] = np.ascontiguousarray(np.stack([pk(c[bi]), pk(cc)], -1))
        maps.append(m)
    return maps


_CACHE = {}


def kernel(x, c, ctx, c_ctx, l0_mod_w, l0_mod_b, l0_na_w_qkv, l0_na_rpb, l0_na_w_o, l0_ln1_g, l0_ln1_b, l0_router_g_w, l0_router_g_b, l0_router_e_w, l0_router_e_b, l0_moe_w13, l0_moe_w2, l0_ln2_g, l0_ln2_b, l1_mod_w, l1_mod_b, l1_cv_w_in, l1_cv_b_in, l1_cv_dw, l1_cv_dw_b, l1_cv_ln_g, l1_cv_ln_b, l1_cv_w_out, l1_cv_b_out, l1_ln1_g, l1_ln1_b, l1_router_g_w, l1_router_g_b, l1_router_e_w, l1_router_e_b, l1_moe_w13, l1_moe_w2, l1_ln2_g, l1_ln2_b, l2_mod_w, l2_mod_b, l2_sw_w_qkv, l2_sw_sink, l2_sw_w_o, l2_ln1_g, l2_ln1_b, l2_router_g_w, l2_router_g_b, l2_router_e_w, l2_router_e_b, l2_moe_w13, l2_moe_w2, l2_ln2_g, l2_ln2_b, l3_mod_w, l3_mod_b, l3_df_w_qkv, l3_df_lambda, l3_df_subln_g, l3_df_w_o, l3_ln1_g, l3_ln1_b, l3_router_g_w, l3_router_g_b, l3_router_e_w, l3_router_e_b, l3_moe_w13, l3_moe_w2, l3_ln2_g, l3_ln2_b):
    inputs = dict(x=x, c=c, ctx=ctx, c_ctx=c_ctx, l0_mod_w=l0_mod_w, l0_mod_b=l0_mod_b, l0_na_w_qkv=l0_na_w_qkv, l0_na_rpb=l0_na_rpb, l0_na_w_o=l0_na_w_o, l0_ln1_g=l0_ln1_g, l0_ln1_b=l0_ln1_b, l0_router_g_w=l0_router_g_w, l0_router_g_b=l0_router_g_b, l0_router_e_w=l0_router_e_w, l0_router_e_b=l0_router_e_b, l0_moe_w13=l0_moe_w13, l0_moe_w2=l0_moe_w2, l0_ln2_g=l0_ln2_g, l0_ln2_b=l0_ln2_b, l1_mod_w=l1_mod_w, l1_mod_b=l1_mod_b, l1_cv_w_in=l1_cv_w_in, l1_cv_b_in=l1_cv_b_in, l1_cv_dw=l1_cv_dw, l1_cv_dw_b=l1_cv_dw_b, l1_cv_ln_g=l1_cv_ln_g, l1_cv_ln_b=l1_cv_ln_b, l1_cv_w_out=l1_cv_w_out, l1_cv_b_out=l1_cv_b_out, l1_ln1_g=l1_ln1_g, l1_ln1_b=l1_ln1_b, l1_router_g_w=l1_router_g_w, l1_router_g_b=l1_router_g_b, l1_router_e_w=l1_router_e_w, l1_router_e_b=l1_router_e_b, l1_moe_w13=l1_moe_w13, l1_moe_w2=l1_moe_w2, l1_ln2_g=l1_ln2_g, l1_ln2_b=l1_ln2_b, l2_mod_w=l2_mod_w, l2_mod_b=l2_mod_b, l2_sw_w_qkv=l2_sw_w_qkv, l2_sw_sink=l2_sw_sink, l2_sw_w_o=l2_sw_w_o, l2_ln1_g=l2_ln1_g, l2_ln1_b=l2_ln1_b, l2_router_g_w=l2_router_g_w, l2_router_g_b=l2_router_g_b, l2_router_e_w=l2_router_e_w, l2_router_e_b=l2_router_e_b, l2_moe_w13=l2_moe_w13, l2_moe_w2=l2_moe_w2, l2_ln2_g=l2_ln2_g, l2_ln2_b=l2_ln2_b, l3_mod_w=l3_mod_w, l3_mod_b=l3_mod_b, l3_df_w_qkv=l3_df_w_qkv, l3_df_lambda=l3_df_lambda, l3_df_subln_g=l3_df_subln_g, l3_df_w_o=l3_df_w_o, l3_ln1_g=l3_ln1_g, l3_ln1_b=l3_ln1_b, l3_router_g_w=l3_router_g_w, l3_router_g_b=l3_router_g_b, l3_router_e_w=l3_router_e_w, l3_router_e_b=l3_router_e_b, l3_moe_w13=l3_moe_w13, l3_moe_w2=l3_moe_w2, l3_ln2_g=l3_ln2_g, l3_ln2_b=l3_ln2_b)
    maps = host_prepare(inputs)
    if "nc" not in _CACHE:
        _CACHE["nc"] = build().nc
    res = run_bass_kernel_spmd(_CACHE["nc"], maps, core_ids=list(range(8)))
    out = np.stack([np.ascontiguousarray(r["outT"].T) for r in res.results], 0)
    return out.astype(np.float32)
```

```python
import math
from contextlib import ExitStack
import numpy as np
import concourse.bass as bass
import concourse.mybir as mybir
from concourse.bass_utils import run_bass_kernel_spmd

F32 = mybir.dt.float32
BF16 = mybir.dt.bfloat16
I32 = mybir.dt.int32
AF = mybir.ActivationFunctionType
ALU = mybir.AluOpType
AX = mybir.AxisListType

D = 2048
L = 2048
C = 256
T = L + C
NT = T // 128
KC = 16
DEPTH = 4
ALPHA = (2.0 * DEPTH) ** 0.25
EPS = 1e-5
EPSP = EPS / (ALPHA * ALPHA)
SCALE = 128 ** -0.5
NEG = -30000.0
BLK = 256
NBLK = (2 * T) // BLK + 32
NSLOT = NBLK * BLK
ND = 40
TB256 = [(i * 256, 256, 0 if i < 8 else 1) for i in range(9)]
TB512 = [(i * 512, 512, 0) for i in range(4)] + [(2048, 256, 1)]


class Bld:
    def __init__(self):
        self.nc = bass.Bass("TRN2", target_bir_lowering=False)
        nc = self.nc
        self.es = ExitStack()
        self.eng = {"pe": nc.tensor, "act": nc.scalar, "dve": nc.vector, "pool": nc.gpsimd, "sp": nc.sync}
        self.esem = {e: self.es.enter_context(nc.semaphore("s_" + e)) for e in self.eng}
        self.ecnt = {e: 0 for e in self.eng}
        self.dsem = [self.es.enter_context(nc.semaphore("d%d" % i)) for i in range(ND)]
        self.dcnt = [0] * ND
        self.dnext = 0
        self.seen = {e: {} for e in self.eng}
        self.lastw = {}
        self.readers = {}
        self.uid = 0

    def _waits(self, e, reads, writes):
        evs = {}
        for k in reads:
            ev = self.lastw.get(k)
            if ev is not None:
                evs[ev[0]] = max(evs.get(ev[0], 0), ev[1])
        for k in writes:
            ev = self.lastw.get(k)
            if ev is not None:
                evs[ev[0]] = max(evs.get(ev[0], 0), ev[1])
            for s, v in self.readers.get(k, {}).items():
                evs[s] = max(evs.get(s, 0), v)
        for s, v in evs.items():
            if s == ("e", "pe") and e == "pe":
                continue
            if self.seen[e].get(s, 0) >= v:
                continue
            self.seen[e][s] = v
            sem = self.esem[s[1]] if s[0] == "e" else self.dsem[s[1]]
            self.eng[e].wait_ge(sem, v)

    def _commit(self, ev, reads, writes):
        for k in writes:
            self.lastw[k] = ev
            self.readers[k] = {}
        for k in reads:
            r = self.readers.setdefault(k, {})
            r[ev[0]] = max(r.get(ev[0], 0), ev[1])

    def op(self, e, fn, reads=(), writes=()):
        self._waits(e, reads, writes)
        self.ecnt[e] += 1
        fn(self.eng[e]).then_inc(self.esem[e], 1)
        self._commit((("e", e), self.ecnt[e]), reads, writes)

    def dma(self, q, fn, reads=(), writes=()):
        i = self.dnext
        self.dnext = (i + 1) % ND
        prev = self.dcnt[i]
        s = ("d", i)
        if prev > 0 and self.seen[q].get(s, 0) < prev:
            self.eng[q].wait_ge(self.dsem[i], prev)
            self.seen[q][s] = prev
        self._waits(q, reads, writes)
        self.dcnt[i] += 16
        fn(self.eng[q]).then_inc(self.dsem[i], 16)
        self._commit((s, self.dcnt[i]), reads, writes)

    def barrier(self):
        for e in self.eng:
            for e2 in self.eng:
                if e2 != e and self.ecnt[e2] > self.seen[e].get(("e", e2), 0):
                    self.eng[e].wait_ge(self.esem[e2], self.ecnt[e2])
                    self.seen[e][("e", e2)] = self.ecnt[e2]
            for i in range(ND):
                if self.dcnt[i] > self.seen[e].get(("d", i), 0):
                    self.eng[e].wait_ge(self.dsem[i], self.dcnt[i])
                    self.seen[e][("d", i)] = self.dcnt[i]
        for e in self.eng:
            if self.ecnt[e] > 0:
                self.eng[e].wait_ge(self.esem[e], self.ecnt[e])

    def sb(self, stack, name, shape, dt):
        self.uid += 1
        return stack.enter_context(self.nc.sbuf_tensor("%s_%d" % (name, self.uid), shape, dt))

    def dram(self, name, shape, dt, kind="Internal"):
        return self.nc.dram_tensor(name, shape, dt, kind=kind).ap()


def vec_layout(kind):
    cols = {}
    n = 0

    def add(name, w):
        nonlocal n
        cols[name] = n
        n += w

    add("mod_b", 96)
    add("ln1_g", 16)
    add("ln1_b", 16)
    add("ln2_g", 16)
    add("ln2_b", 16)
    add("rb", 36)
    if kind == 1:
        add("b_in", 32)
        add("dw", 31 * 16)
        add("dw_b", 16)
        add("cln_g", 16)
        add("cln_b", 16)
        add("b_out", 16)
    if kind == 2:
        add("sink", 16)
    if kind == 3:
        add("subg", 2)
        add("lam", 4)
    return cols, n


def pk(v):
    v = np.asarray(v, np.float32)
    return np.ascontiguousarray(v.reshape(-1, 128).T)


def build(dbg=None):
    b = Bld()
    nc = b.nc
    G = ExitStack()
    okind = "ExternalOutput"
    skind = "ExternalOutput" if dbg else "Internal"

    xT_in = b.dram("xT", [D, T], F32, "ExternalInput")
    cT_in = b.dram("cT", [128, KC, 2], F32, "ExternalInput")
    rope_in = b.dram("rope", [2, 128, T], F32, "ExternalInput")
    cst_in = b.dram("cst", [128, 4, 128], F32, "ExternalInput")
    cst2_in = b.dram("cst2", [128, 66], F32, "ExternalInput")
    swm_in = b.dram("swm", [6, 128, 512], F32, "ExternalInput")
    nab_in = b.dram("nab", [16, 20, 128, 512], F32, "ExternalInput")
    vec_in, mod_w, rw_in, w13_in, w2_in = [], [], [], [], []
    VL = [vec_layout(i) for i in range(4)]
    for i in range(4):
        vec_in.append(b.dram("vec%d" % i, [128, VL[i][1]], F32, "ExternalInput"))
        mod_w.append(b.dram("modw%d" % i, [D, 6 * D], F32, "ExternalInput"))
        rw_in.append(b.dram("rw%d" % i, [128, KC, 36], F32, "ExternalInput"))
        w13_in.append([b.dram("w13_%d_%d" % (i, m), [32 * 128, 4096], F32, "ExternalInput") for m in range(4)])
        w2_in.append([b.dram("w2_%d_%d" % (i, m), [32 * 128, 2048], F32, "ExternalInput") for m in range(4)])
    wqkv0 = b.dram("wqkv0", [D, 6144], F32, "ExternalInput")
    wo0 = b.dram("wo0", [D, D], F32, "ExternalInput")
    win1 = b.dram("win1", [D, 4096], F32, "ExternalInput")
    wo1 = b.dram("wo1", [D, D], F32, "ExternalInput")
    wqkv2 = b.dram("wqkv2", [D, 3072], F32, "ExternalInput")
    wo2 = b.dram("wo2", [D, D], F32, "ExternalInput")
    wqkv3 = b.dram("wqkv3", [D, 6144], F32, "ExternalInput")
    wo3 = b.dram("wo3", [D, D], F32, "ExternalInput")
    out_T = b.dram("outT", [D, L], F32, okind)

    XA = b.dram("XA", [D, T], F32, skind)
    XB = b.dram("XB", [D, T], F32, skind)
    QT = b.dram("QT", [D, T], BF16, skind)
    KT = b.dram("KT", [D, T], BF16, skind)
    VV = b.dram("VV", [T, D], BF16, skind)
    OT = b.dram("OT", [D, T], BF16, skind)
    CT = b.dram("CT", [D, T], F32, skind)
    HTOK = b.dram("HTOK", [T, D], BF16, skind)
    XS = b.dram("XS", [NSLOT, D], BF16, skind)
    YS = b.dram("YS", [NSLOT, D], BF16, skind)
    RDBG = b.dram("RDBG", [128, NT * 36], F32, skind)

    cst = b.sb(G, "cst", [128, 4, 128], F32)
    cst2 = b.sb(G, "cst2", [128, 66], F32)
    utb = b.sb(G, "utb", [128, 128], BF16)
    identf = cst[:, 0, :]
    identb = b.sb(G, "identb", [128, 128], BF16)
    onesb = b.sb(G, "onesb", [128, 128], BF16)
    permb = b.sb(G, "permb", [128, 128], BF16)
    modv = b.sb(G, "modv", [128, 4, 6, 16, 2], F32)
    vecs = [b.sb(G, "vec%d" % i, [128, VL[i][1]], F32) for i in range(4)]
    der = b.sb(G, "der", [128, 6, 16, 2], F32)
    tmpd = b.sb(G, "tmpd", [128, 16, 2], F32)
    lg = b.sb(G, "lg", [128, NT, 36], F32)
    slot_i = b.sb(G, "slot_i", [128, 2, NT], I32)
    gate = b.sb(G, "gate", [128, 2, NT], F32)
    widx = b.sb(G, "widx", [128, NBLK], I32)
    PS = [G.enter_context(nc.psum_tensor("ps%d" % i, [128, 512], F32)) for i in range(8)]
    PK = ["ps%d" % i for i in range(8)]

    b.dma("sp", lambda q: q.dma_start(out=cst[:], in_=cst_in), writes=["cst"])
    for i in range(4):
        b.dma("sp", lambda q, i=i: q.dma_start(out=vecs[i][:], in_=vec_in[i]), writes=["vec%d" % i])
    b.op("dve", lambda e: e.tensor_copy(out=identb[:], in_=cst[:, 0, :]), reads=["cst"], writes=["identb"])
    b.op("dve", lambda e: e.tensor_copy(out=onesb[:], in_=cst[:, 1, :]), reads=["cst"], writes=["onesb"])
    b.op("dve", lambda e: e.tensor_copy(out=permb[:], in_=cst[:, 2, :]), reads=["cst"], writes=["permb"])
    b.op("dve", lambda e: e.tensor_copy(out=utb[:], in_=cst[:, 3, :]), reads=["cst"], writes=["utb"])
    b.dma("sp", lambda q: q.dma_start(out=cst2[:], in_=cst2_in), writes=["cst2"])

    def vcol(i, name, c=0, w=1):
        o = VL[i][0][name] + c
        return vecs[i][:, o:o + w]

    def phase_mod():
        with ExitStack() as S:
            mw = [b.sb(S, "mw", [128, KC, 1024], BF16) for _ in range(2)]
            cT = b.sb(S, "cT", [128, KC, 2], F32)
            sc = b.sb(S, "sc", [128, KC, 2], BF16)
            b.dma("sp", lambda q: q.dma_start(out=cT[:], in_=cT_in), writes=["cT"])
            b.op("act", lambda e: e.activation(out=sc[:], in_=cT[:], func=AF.Silu), reads=["cT"], writes=["sc"])
            n = 0
            for li in range(4):
                src = mod_w[li].rearrange("(c p) n -> p c n", p=128)
                for pc in range(12):
                    buf = mw[n % 2]
                    bk = "mw%d" % (n % 2)
                    n += 1
                    b.dma("pool", lambda q, buf=buf, pc=pc, src=src: q.dma_start(out=buf[:], in_=src[:, :, pc * 1024:(pc + 1) * 1024]),
                          writes=[bk])
                    for j in range(8):
                        oc = pc * 8 + j
                        pi = oc % 2
                        for k in range(KC):
                            b.op("pe", lambda e, k=k, j=j, buf=buf, pi=pi: e.matmul(PS[pi][:, 0:2], lhsT=buf[:, k, j * 128:(j + 1) * 128],
                                                                                      rhs=sc[:, k, :], start=(k == 0), stop=(k == KC - 1)),
                                 reads=[bk, "sc"], writes=[PK[pi]])
                        b.op("dve", lambda e, li=li, oc=oc, pi=pi: e.tensor_scalar(out=modv[:, li, oc // 16, oc % 16, :], in0=PS[pi][:, 0:2],
                                                                                    scalar1=vcol(li, "mod_b", oc), scalar2=None, op0=ALU.add),
                             reads=[PK[pi], "vec%d" % li], writes=["modv"])
        b.barrier()

    def derive(li):
        m = lambda k: modv[:, li, k]
        R = ["modv", "vec%d" % li]
        b.op("dve", lambda e: e.tensor_scalar(out=der[:, 0], in0=m(1), scalar1=1.0, scalar2=None, op0=ALU.add), reads=R, writes=["der"])
        b.op("dve", lambda e: e.tensor_copy(out=der[:, 1], in_=m(0)), reads=R, writes=["der"])
        b.op("dve", lambda e: e.tensor_scalar(out=der[:, 2], in0=m(2), scalar1=1.0 / ALPHA, scalar2=None, op0=ALU.mult), reads=R, writes=["der"])
        b.op("dve", lambda e: e.tensor_scalar(out=tmpd[:], in0=m(4), scalar1=1.0, scalar2=None, op0=ALU.add), reads=R, writes=["tmpd"])
        for j in range(2):
            b.op("dve", lambda e, j=j: e.tensor_tensor(out=der[:, 3, :, j], in0=tmpd[:, :, j], in1=vcol(li, "ln1_g", 0, 16), op=ALU.mult),
                 reads=R + ["tmpd"], writes=["der"])
            b.op("dve", lambda e, j=j: e.tensor_tensor(out=der[:, 4, :, j], in0=tmpd[:, :, j], in1=vcol(li, "ln1_b", 0, 16), op=ALU.mult),
                 reads=R + ["tmpd"], writes=["der"])
        b.op("dve", lambda e: e.tensor_tensor(out=der[:, 4], in0=der[:, 4], in1=m(3), op=ALU.add), reads=R + ["der"], writes=["der"])
        b.op("dve", lambda e: e.tensor_scalar(out=der[:, 5], in0=m(5), scalar1=1.0 / ALPHA, scalar2=None, op0=ALU.mult), reads=R, writes=["der"])

    def build_u(S, XT):
        uT = b.sb(S, "uT", [128, KC, T], BF16)
        xs_ = [b.sb(S, "xst", [128, KC, 256], F32) for _ in range(2)]
        src = XT.rearrange("(c p) t -> p c t", p=128)
        for n, (t0, tn, j) in enumerate(TB256):
            xb = xs_[n % 2]
            kx = "xst%d" % (n % 2)
            b.dma("sp", lambda q, xb=xb, t0=t0: q.dma_start(out=xb[:], in_=src[:, :, t0:t0 + 256]), writes=[kx])
            for c in range(KC):
                b.op("act", lambda e, xb=xb, c=c, t0=t0, j=j: e.activation(out=uT[:, c, t0:t0 + 256], in_=xb[:, c, :], func=AF.Identity,
                                                                       scale=der[:, 0, c, j:j + 1], bias=der[:, 1, c, j:j + 1]),
                     reads=[kx, "der"], writes=["uT"])
        return uT

    def proj_fm(S, uT, W, oc0, noc, dstT, rope):
        wq = [b.sb(S, "wq", [128, KC, 512], BF16) for _ in range(2)]
        stg = [b.sb(S, "qstg", [128, T], BF16) for _ in range(2)]
        if rope:
            rp = b.sb(S, "rp", [128, 2, T], F32)
            b.dma("sp", lambda q: q.dma_start(out=rp[:], in_=rope_in.rearrange("a p t -> p a t")), writes=["rp"])
            qb = [b.sb(S, "qb", [128, 512], BF16) for _ in range(2)]
            t1 = [b.sb(S, "t1", [128, 512], F32) for _ in range(2)]
            t2 = [b.sb(S, "t2", [128, 512], F32) for _ in range(2)]
        src = W.rearrange("(c p) n -> p c n", p=128)
        npc = (noc + 3) // 4
        cnt = 0
        for pc in range(npc):
            wb = wq[pc % 2]
            kw = "wq%d" % (pc % 2)
            c0 = (oc0 + pc * 4) * 128
            ncol = min(4, noc - pc * 4) * 128
            b.dma("pool", lambda q, wb=wb, c0=c0, ncol=ncol: q.dma_start(out=wb[:, :, 0:ncol], in_=src[:, :, c0:c0 + ncol]), writes=[kw])
            for jj in range(ncol // 128):
                oc = pc * 4 + jj
                sg = stg[oc % 2]
                ks = "qstg%d" % (oc % 2)
                for (t0, tn, _) in TB512:
                    pi = cnt % 2
                    r = cnt % 2
                    cnt += 1
                    for k in range(KC):
                        b.op("pe", lambda e, k=k, jj=jj, wb=wb, pi=pi, t0=t0, tn=tn: e.matmul(PS[pi][:, 0:tn], lhsT=wb[:, k, jj * 128:(jj + 1) * 128],
                                                                                              rhs=uT[:, k, t0:t0 + tn], start=(k == 0), stop=(k == KC - 1)),
                             reads=[kw, "uT"], writes=[PK[pi]])
                    if not rope:
                        b.op("act", lambda e, sg=sg, pi=pi, t0=t0, tn=tn: e.activation(out=sg[:, t0:t0 + tn], in_=PS[pi][:, 0:tn], func=AF.Copy),
                             reads=[PK[pi]], writes=[ks])
                    else:
                        b.op("act", lambda e, r=r, pi=pi, tn=tn: e.activation(out=qb[r][:, 0:tn], in_=PS[pi][:, 0:tn], func=AF.Copy),
                             reads=[PK[pi]], writes=["qb%d" % r])
                        b.op("pe", lambda e, r=r, tn=tn: e.matmul(PS[2 + r][:, 0:tn], lhsT=permb[:], rhs=qb[r][:, 0:tn], start=True, stop=True),
                             reads=["permb", "qb%d" % r], writes=[PK[2 + r]])
                        b.op("dve", lambda e, r=r, t0=t0, tn=tn: e.tensor_tensor(out=t1[r][:, 0:tn], in0=qb[r][:, 0:tn], in1=rp[:, 0, t0:t0 + tn], op=ALU.mult),
                             reads=["qb%d" % r, "rp"], writes=["t1%d" % r])
                        b.op("dve", lambda e, r=r, t0=t0, tn=tn: e.tensor_tensor(out=t2[r][:, 0:tn], in0=PS[2 + r][:, 0:tn], in1=rp[:, 1, t0:t0 + tn], op=ALU.mult),
                             reads=[PK[2 + r], "rp"], writes=["t2%d" % r])
                        b.op("dve", lambda e, r=r, sg=sg, t0=t0, tn=tn: e.tensor_tensor(out=sg[:, t0:t0 + tn], in0=t1[r][:, 0:tn], in1=t2[r][:, 0:tn], op=ALU.add),
                             reads=["t1%d" % r, "t2%d" % r], writes=[ks])
                b.dma("sp", lambda q, sg=sg, oc=oc: q.dma_start(out=dstT[oc * 128:(oc + 1) * 128, :], in_=sg[:]), reads=[ks], writes=[("dstT", id(dstT))])

    def proj_tm(S, uT, W, c0, ncols, dst):
        wq = [b.sb(S, "wv", [128, KC, 512], BF16) for _ in range(2)]
        stg = [b.sb(S, "vstg", [128, 512], BF16) for _ in range(3)]
        src = W.rearrange("(c p) n -> p c n", p=128)
        cnt = 0
        for pc in range(ncols // 512):
            wb = wq[pc % 2]
            kw = "wv%d" % (pc % 2)
            b.dma("pool", lambda q, wb=wb, pc=pc: q.dma_start(out=wb[:], in_=src[:, :, c0 + pc * 512:c0 + (pc + 1) * 512]), writes=[kw])
            for i in range(NT):
                pi = 4 + cnt % 2
                sg = stg[cnt % 3]
                ks = "vstg%d" % (cnt % 3)
                cnt += 1
                for k in range(KC):
                    b.op("pe", lambda e, k=k, wb=wb, pi=pi, i=i: e.matmul(PS[pi][:], lhsT=uT[:, k, i * 128:(i + 1) * 128], rhs=wb[:, k, :],
                                                                           start=(k == 0), stop=(k == KC - 1)),
                         reads=[kw, "uT"], writes=[PK[pi]])
                b.op("act", lambda e, sg=sg, pi=pi: e.activation(out=sg[:], in_=PS[pi][:], func=AF.Copy), reads=[PK[pi]], writes=[ks])
                b.dma("sp", lambda q, sg=sg, i=i, pc=pc: q.dma_start(out=dst[i * 128:(i + 1) * 128, pc * 512:(pc + 1) * 512], in_=sg[:]),
                      reads=[ks], writes=[("dst", id(dst))])

    def phase_premix_attn(li, XT, W, nq, nk, nv, rope):
        with ExitStack() as S:
            uT = build_u(S, XT)
            with ExitStack() as S2:
                proj_fm(S2, uT, W, 0, nq, QT, rope)
            b.barrier()
            with ExitStack() as S2:
                proj_fm(S2, uT, W, nq, nk, KT, rope)
            b.barrier()
            with ExitStack() as S2:
                proj_tm(S2, uT, W, (nq + nk) * 128, nv, VV)
        b.barrier()

    def attn_head_loop(S, heads, Dv, qblocks_fn, finish_fn, extra_den=None):
        nd = Dv // 128
        qh = [b.sb(S, "qh", [128, T], BF16) for _ in range(2)]
        kh = [b.sb(S, "kh", [128, T], BF16) for _ in range(2)]
        vh = [b.sb(S, "vh", [128, NT, Dv], BF16) for _ in range(2)]
        pt = [b.sb(S, "pt", [128, 512], BF16) for _ in range(4)]
        pt0 = [b.sb(S, "pt0", [128, 512], BF16) for _ in range(2)]
        sb_ = [b.sb(S, "sbias", [128, 512], F32) for _ in range(2)]
        bt = [b.sb(S, "btile", [128, 512], F32) for _ in range(4)]
        rec = [b.sb(S, "rec", [128, 512], F32) for _ in range(2)]
        swm = b.sb(S, "swm", [128, 6, 512], BF16)
        b.dma("pool", lambda q: q.dma_start(out=swm[:], in_=swm_in.rearrange("a p t -> p a t")), writes=["swm"])
        vsrc = VV.rearrange("(n p) d -> p n d", p=128)
        ACC = [([2, 3], 4), ([5, 6], 7)]
        items = []
        qbn = 0
        for hn, (qrow, krow, vcol_, tag) in enumerate(heads):
            for (q0, nq, chunks) in qblocks_fn(tag):
                for ci, ch in enumerate(chunks):
                    items.append(dict(hn=hn, qrow=qrow, krow=krow, vcol=vcol_, tag=tag, q0=q0, nq=nq, ci=ci, nch=len(chunks), ch=ch,
                                      first=(q0 == 0 and ci == 0), acc=qbn % 2))
                qbn += 1
        cnt = {"s": 0, "p": 0, "b": 0}

        def stageA(it):
            hn = it["hn"]
            r2 = hn % 2
            q0, nq = it["q0"], it["nq"]
            k0, kind, arg = it["ch"]
            si = cnt["s"] % 2
            cnt["s"] += 1
            b.op("pe", lambda e: e.matmul(PS[si][:, 0:nq], lhsT=kh[r2][:, k0:k0 + 128], rhs=qh[r2][:, q0:q0 + nq], start=True, stop=True),
                 reads=["qh%d" % r2, "kh%d" % r2], writes=[PK[si]])
            pi = cnt["p"] % 4
            cnt["p"] += 1
            P = pt[pi]
            kp = "pt%d" % pi
            it["P"], it["kp"] = P, kp
            if kind == "bias":
                bi = cnt["b"] % 4
                bj = cnt["b"] % 2
                cnt["b"] += 1
                b.dma("sp", lambda q: q.dma_start(out=bt[bi][:], in_=nab_in[arg[0], arg[1]]), writes=["bt%d" % bi])
                b.op("dve", lambda e: e.scalar_tensor_tensor(out=sb_[bj][:, 0:nq], in0=PS[si][:, 0:nq], scalar=SCALE, in1=bt[bi][:, 0:nq], op0=ALU.mult, op1=ALU.add),
                     reads=[PK[si], "bt%d" % bi], writes=["sb%d" % bj])
                b.op("act", lambda e: e.activation(out=P[:, 0:nq], in_=sb_[bj][:, 0:nq], func=AF.Exp), reads=["sb%d" % bj], writes=[kp])
            elif kind == "mask":
                bj = cnt["b"] % 2
                cnt["b"] += 1
                b.op("act", lambda e: e.activation(out=pt0[bj][:, 0:nq], in_=PS[si][:, 0:nq], func=AF.Exp, scale=SCALE), reads=[PK[si]], writes=["pt0%d" % bj])
                b.op("dve", lambda e: e.tensor_tensor(out=P[:, 0:nq], in0=pt0[bj][:, 0:nq], in1=swm[:, arg, 0:nq], op=ALU.mult), reads=["pt0%d" % bj, "swm"], writes=[kp])
            else:
                b.op("act", lambda e: e.activation(out=P[:, 0:nq], in_=PS[si][:, 0:nq], func=AF.Exp, scale=SCALE), reads=[PK[si]], writes=[kp])

        def stageB(it):
            hn = it["hn"]
            r2 = hn % 2
            q0, nq, ci, nch = it["q0"], it["nq"], it["ci"], it["nch"]
            P, kp = it["P"], it["kp"]
            ob, rb = ACC[it["acc"]]
            kt = it["ch"][0] // 128
            for dv in range(nd):
                b.op("pe", lambda e, dv=dv: e.matmul(PS[ob[dv]][:, 0:nq], lhsT=vh[r2][:, kt, dv * 128:(dv + 1) * 128], rhs=P[:, 0:nq], start=(ci == 0), stop=(ci == nch - 1)),
                     reads=[kp, "vh%d" % r2], writes=[PK[ob[dv]]])
            b.op("pe", lambda e: e.matmul(PS[rb][:, 0:nq], lhsT=onesb[:], rhs=P[:, 0:nq], start=(ci == 0), stop=(ci == nch - 1)), reads=[kp, "onesb"], writes=[PK[rb]])
            if ci == nch - 1:
                ri = it["acc"]
                tag = it["tag"]
                if extra_den is not None:
                    b.op("dve", lambda e: e.tensor_scalar(out=rec[ri][:, 0:nq], in0=PS[rb][:, 0:nq], scalar1=extra_den(tag), scalar2=None, op0=ALU.add),
                         reads=[PK[rb], "esink"], writes=["rec%d" % ri])
                    b.op("dve", lambda e: e.reciprocal(out=rec[ri][:, 0:nq], in_=rec[ri][:, 0:nq]), reads=["rec%d" % ri], writes=["rec%d" % ri])
                else:
                    b.op("dve", lambda e: e.reciprocal(out=rec[ri][:, 0:nq], in_=PS[rb][:, 0:nq]), reads=[PK[rb]], writes=["rec%d" % ri])
                finish_fn(hn, tag, q0, nq, rec[ri], "rec%d" % ri, ob, rb)

        def load_head(hn):
            if hn >= len(heads):
                return
            qrow, krow, vcol_, tag = heads[hn]
            r2 = hn % 2
            b.dma("sp", lambda q: q.dma_start(out=qh[r2][:], in_=QT[qrow:qrow + 128, :]), reads=[("dstT", id(QT))], writes=["qh%d" % r2])
            b.dma("sp", lambda q: q.dma_start(out=kh[r2][:], in_=KT[krow:krow + 128, :]), reads=[("dstT", id(KT))], writes=["kh%d" % r2])
            b.dma("sp", lambda q: q.dma_start(out=vh[r2][:], in_=vsrc[:, :, vcol_:vcol_ + Dv]), reads=[("dst", id(VV))], writes=["vh%d" % r2])

        load_head(0)
        load_head(1)
        LOOK = 2
        for i in range(len(items) + LOOK):
            if i < len(items):
                stageA(items[i])
            if i - LOOK >= 0:
                itb = items[i - LOOK]
                stageB(itb)
                if i - LOOK + 1 == len(items) or items[i - LOOK + 1]["hn"] != itb["hn"]:
                    load_head(itb["hn"] + 2)

    def simple_finish(S):
        ostg = [b.sb(S, "ostg", [128, T], BF16) for _ in range(2)]

        def fin(hn, tag, q0, nq, rec, krec, ob, rb):
            og = ostg[hn % 2]
            ko = "ostg%d" % (hn % 2)
            b.op("dve", lambda e: e.tensor_tensor(out=og[:, q0:q0 + nq], in0=PS[ob[0]][:, 0:nq], in1=rec[:, 0:nq], op=ALU.mult),
                 reads=[PK[ob[0]], krec], writes=[ko])
            if q0 + nq == T:
                b.dma("sp", lambda q: q.dma_start(out=OT[tag * 128:(tag + 1) * 128, :], in_=og[:]), reads=[ko], writes=[("dstT", id(OT))])
        return fin

    def na_qblocks(h):
        res = []
        for bq in range(4):
            if bq == 0:
                kcs = [(kc, kc) for kc in range(6)]
            elif bq == 3:
                kcs = [(kc, 14 + kc - 10) for kc in range(10, 16)]
            else:
                kcs = [(4 * bq - 2 + j, 6 + j) for j in range(8)]
            ch = [(kc * 128, "bias", (h, ti)) for kc, ti in kcs]
            ch += [(2048, "none", None), (2176, "none", None)]
            res.append((bq * 512, 512, ch))
        res.append((2048, 256, [(2048, "none", None), (2176, "none", None)]))
        return res

    def phase_attn_na():
        with ExitStack() as S:
            heads = [(h * 128, h * 128, h * 128, h) for h in range(16)]
            attn_head_loop(S, heads, 128, na_qblocks, simple_finish(S))
        b.barrier()

    def ln_phase(li, XT, XTn, y_src, gidx, lng, lnb, with_router, final=False):
        with ExitStack() as S:
            xz = [b.sb(S, "xz", [128, KC, 256], F32) for _ in range(2)]
            x1 = [b.sb(S, "x1", [128, KC, 256], F32) for _ in range(2)]
            zb = [b.sb(S, "zb", [128, 256], BF16) for _ in range(3)]
            zq = [b.sb(S, "zq", [128, 256], BF16) for _ in range(3)]
            mean = b.sb(S, "mean", [128, 256], F32)
            rstd = b.sb(S, "rstd", [128, 256], F32)
            var = b.sb(S, "var", [128, 256], F32)
            hst = [b.sb(S, "hst", [128, 512], BF16) for _ in range(3)]
            if with_router:
                rw = b.sb(S, "rw", [128, KC, 36], F32)
                b.dma("sp", lambda q: q.dma_start(out=rw[:], in_=rw_in[li]), writes=["rw"])
            zfn = y_src(S)
            src = XT.rearrange("(c p) t -> p c t", p=128)
            dstv = XTn.rearrange("(c p) t -> p c t", p=128) if not final else out_T.rearrange("(c p) t -> p c t", p=128)
            cnt = {"z": 0, "h": 0}
            pend = {"stats": None, "tr": None}
            SBK = [(2, 3), (6, 7)]

            def loadX(tbi):
                t0 = TB256[tbi][0]
                X = xz[tbi % 2]
                b.dma("sp", lambda q: q.dma_start(out=X[:], in_=src[:, :, t0:t0 + 256]), reads=[("xt", id(XT))], writes=["xz%d" % (tbi % 2)])

            def P1chunk(tbi, c):
                t0, tn, j = TB256[tbi]
                X = xz[tbi % 2]
                kx = "xz%d" % (tbi % 2)
                s1, s2 = SBK[tbi % 2]
                zfn(tbi, t0, j, c, X, kx)
                zi = cnt["z"] % 3
                cnt["z"] += 1
                b.op("act", lambda e: e.activation(out=zb[zi][:], in_=X[:, c, :], func=AF.Copy), reads=[kx], writes=["zb%d" % zi])
                b.op("act", lambda e: e.activation(out=zq[zi][:], in_=X[:, c, :], func=AF.Square), reads=[kx], writes=["zq%d" % zi])
                def stats(c=c, zi=zi, s1=s1, s2=s2):
                    b.op("pe", lambda e: e.matmul(PS[s1][:, 0:256], lhsT=onesb[:], rhs=zb[zi][:], start=(c == 0), stop=(c == KC - 1)),
                         reads=["zb%d" % zi, "onesb"], writes=[PK[s1]])
                    b.op("pe", lambda e: e.matmul(PS[s2][:, 0:256], lhsT=onesb[:], rhs=zq[zi][:], start=(c == 0), stop=(c == KC - 1)),
                         reads=["zq%d" % zi, "onesb"], writes=[PK[s2]])
                if pend["stats"] is not None:
                    pend["stats"]()
                pend["stats"] = stats
                if c == KC - 1:
                    pend["stats"]()
                    pend["stats"] = None

            def P2pre(tbi):
                s1, s2 = SBK[tbi % 2]
                b.op("dve", lambda e: e.tensor_scalar(out=mean[:], in0=PS[s1][:, 0:256], scalar1=1.0 / D, scalar2=None, op0=ALU.mult), reads=[PK[s1]], writes=["mean"])
                b.op("dve", lambda e: e.tensor_tensor(out=var[:], in0=mean[:], in1=mean[:], op=ALU.mult), reads=["mean"], writes=["var"])
                b.op("dve", lambda e: e.scalar_tensor_tensor(out=var[:], in0=PS[s2][:, 0:256], scalar=1.0 / D, in1=var[:], op0=ALU.mult, op1=ALU.subtract),
                     reads=[PK[s2], "var"], writes=["var"])
                b.op("dve", lambda e: e.tensor_scalar(out=var[:], in0=var[:], scalar1=EPSP, scalar2=None, op0=ALU.add), reads=["var"], writes=["var"])
                b.op("act", lambda e: e.activation(out=var[:], in_=var[:], func=AF.Sqrt), reads=["var"], writes=["var"])
                b.op("dve", lambda e: e.reciprocal(out=rstd[:], in_=var[:]), reads=["var"], writes=["rstd"])

            def P2chunk(tbi, c):
                t0, tn, j = TB256[tbi]
                X = xz[tbi % 2]
                kx = "xz%d" % (tbi % 2)
                X1 = x1[tbi % 2]
                k1 = "x1%d" % (tbi % 2)
                b.op("dve", lambda e: e.tensor_tensor(out=X[:, c, :], in0=X[:, c, :], in1=mean[:], op=ALU.subtract), reads=[kx, "mean"], writes=[kx])
                b.op("dve", lambda e: e.tensor_tensor(out=X[:, c, :], in0=X[:, c, :], in1=rstd[:], op=ALU.mult), reads=[kx, "rstd"], writes=[kx])
                b.op("act", lambda e: e.activation(out=X1[:, c, :], in_=X[:, c, :], func=AF.Identity, scale=vcol(li, lng, c), bias=vcol(li, lnb, c)),
                     reads=[kx, "vec%d" % li], writes=[k1])
                if with_router:
                    b.op("act", lambda e: e.activation(out=X[:, c, :], in_=X[:, c, :], func=AF.Identity, scale=der[:, 3, c, j:j + 1], bias=der[:, 4, c, j:j + 1]),
                         reads=[kx, "der"], writes=[kx])
                    def trgroup(g4=c // 4, X=X, kx=kx, tbi=tbi):
                        for tt in range(2):
                            ti = tbi * 2 + tt
                            hi = cnt["h"] % 3
                            cnt["h"] += 1
                            for cc in range(4):
                                c2 = g4 * 4 + cc
                                b.op("pe", lambda e, c2=c2, cc=cc, tt=tt: e.transpose(PS[5][:, cc * 128:(cc + 1) * 128], X[:, c2, tt * 128:(tt + 1) * 128], identf),
                                     reads=[kx, "cst"], writes=[PK[5]])
                            b.op("act", lambda e, hi=hi: e.activation(out=hst[hi][:], in_=PS[5][:], func=AF.Copy), reads=[PK[5]], writes=["hst%d" % hi])
                            b.dma("sp", lambda q, hi=hi, ti=ti: q.dma_start(out=HTOK[ti * 128:(ti + 1) * 128, g4 * 512:(g4 + 1) * 512], in_=hst[hi][:]),
                                  reads=["hst%d" % hi], writes=["HTOK"])
                    if pend["tr"] is not None:
                        pend["tr"]()
                        pend["tr"] = None
                    if c % 4 == 3:
                        pend["tr"] = trgroup
                        if c == KC - 1:
                            pend["tr"]()
                            pend["tr"] = None

            def P2post(tbi):
                t0, tn, j = TB256[tbi]
                X = xz[tbi % 2]
                kx = "xz%d" % (tbi % 2)
                X1 = x1[tbi % 2]
                k1 = "x1%d" % (tbi % 2)
                if final:
                    if j == 0:
                        b.dma("sp", lambda q: q.dma_start(out=dstv[:, :, t0:t0 + 256], in_=X1[:]), reads=[k1], writes=["outT"])
                else:
                    b.dma("sp", lambda q: q.dma_start(out=dstv[:, :, t0:t0 + 256], in_=X1[:]), reads=[k1], writes=[("xt", id(XTn))])
                if with_router:
                    for tt in range(2):
                        ti = tbi * 2 + tt
                        for k in range(KC):
                            b.op("pe", lambda e, k=k, tt=tt: e.matmul(PS[4][:, 0:36], lhsT=X[:, k, tt * 128:(tt + 1) * 128], rhs=rw[:, k, :],
                                                                       start=(k == 0), stop=(k == KC - 1)), reads=[kx, "rw"], writes=[PK[4]])
                        b.op("dve", lambda e, ti=ti: e.tensor_tensor(out=lg[:, ti, :], in0=PS[4][:, 0:36], in1=vcol(li, "rb", 0, 36), op=ALU.add),
                             reads=[PK[4], "vec%d" % li], writes=["lg"])

            nbk = len(TB256)
            loadX(0)
            for c in range(KC):
                P1chunk(0, c)
            for tbi in range(nbk):
                if tbi + 1 < nbk:
                    loadX(tbi + 1)
                P2pre(tbi)
                for c in range(KC):
                    if tbi + 1 < nbk:
                        P1chunk(tbi + 1, c)
                    P2chunk(tbi, c)
                P2post(tbi)
        b.barrier()

    def wo_ysrc(Wo, gidx, bias_col=None, li=None):
        def mk(S):
            wo = b.sb(S, "wo", [128, KC, D], BF16)
            src = Wo.rearrange("(c p) n -> p c n", p=128)
            for hlf in range(4):
                b.dma("pool", lambda q, hlf=hlf: q.dma_start(out=wo[:, :, hlf * 512:(hlf + 1) * 512], in_=src[:, :, hlf * 512:(hlf + 1) * 512]), writes=["wo"])
            ob = [b.sb(S, "ob", [128, KC, 256], BF16) for _ in range(2)]
            ysb = [b.sb(S, "ysb", [128, 256], F32) for _ in range(2)]
            osrc = OT.rearrange("(c p) t -> p c t", p=128)
            st = {"n": 0}

            def zfn(tbi, t0, j, c, X, kx):
                O = ob[tbi % 2]
                ko = "ob%d" % (tbi % 2)
                if c == 0:
                    b.dma("sp", lambda q: q.dma_start(out=O[:], in_=osrc[:, :, t0:t0 + 256]), reads=[("dstT", id(OT))], writes=[ko])
                pi = st["n"] % 2
                st["n"] += 1
                for k in range(KC):
                    b.op("pe", lambda e, k=k: e.matmul(PS[pi][:, 0:256], lhsT=wo[:, k, c * 128:(c + 1) * 128], rhs=O[:, k, :], start=(k == 0), stop=(k == KC - 1)),
                         reads=["wo", ko], writes=[PK[pi]])
                if bias_col is None:
                    b.op("dve", lambda e: e.scalar_tensor_tensor(out=X[:, c, :], in0=PS[pi][:, 0:256], scalar=der[:, gidx, c, j:j + 1], in1=X[:, c, :],
                                                                  op0=ALU.mult, op1=ALU.add), reads=[PK[pi], kx, "der"], writes=[kx])
                else:
                    Y = ysb[pi]
                    b.op("act", lambda e: e.activation(out=Y[:], in_=PS[pi][:, 0:256], func=AF.Identity, bias=vcol(li, bias_col, c)),
                         reads=[PK[pi], "vec%d" % li], writes=["ysb%d" % pi])
                    b.op("dve", lambda e: e.scalar_tensor_tensor(out=X[:, c, :], in0=Y[:], scalar=der[:, gidx, c, j:j + 1], in1=X[:, c, :],
                                                                  op0=ALU.mult, op1=ALU.add), reads=["ysb%d" % pi, kx, "der"], writes=[kx])
            return zfn
        return mk


    def phase_moe_route(li):
        with ExitStack() as S:
            t18 = lambda n: b.sb(S, n, [128, NT], F32)
            gmax, gs, gp, m1, m2, rr, den = [t18(n) for n in ["gmax", "gs", "gp", "m1", "m2", "rr", "den"]]
            d4 = b.sb(S, "d4", [128, NT, 4], F32)
            og = b.sb(S, "og", [128, NT, 4], F32)
            pen = b.sb(S, "pen", [128, NT, 4], F32)
            lm = b.sb(S, "lm", [128, NT, 32], F32)
            lm2 = b.sb(S, "lm2", [128, NT, 32], F32)
            o1 = b.sb(S, "o1", [128, NT, 32], F32)
            o2 = b.sb(S, "o2", [128, NT, 32], F32)
            Mb = b.sb(S, "Mb", [128, NT, 32], BF16)
            rk = b.sb(S, "rk", [128, NT, 32], F32)
            pr = b.sb(S, "pr", [128, NT, 32], F32)
            cnt = b.sb(S, "cnt", [128, 32], F32)
            nb_ = b.sb(S, "nb", [128, 32], F32)
            pe_ = b.sb(S, "pe", [128, 32], F32)
            pst = b.sb(S, "pst", [128, 32], F32)
            tm = b.sb(S, "tm", [128, 32], F32)
            be = b.sb(S, "be", [128, 64], F32)
            slf = b.sb(S, "slf", [128, 2, NT], F32)
            V = lambda e, fn, r, w: b.op(e, fn, reads=r, writes=w)
            V("dve", lambda e: e.tensor_reduce(out=gmax[:], in_=lg[:, :, 0:4], axis=AX.X, op=ALU.max), ["lg"], ["gmax"])
            for g in range(4):
                V("dve", lambda e, g=g: e.tensor_tensor(out=og[:, :, g], in0=lg[:, :, g], in1=gmax[:], op=ALU.is_equal), ["lg", "gmax"], ["og"])
                V("dve", lambda e, g=g: e.tensor_tensor(out=d4[:, :, g], in0=lg[:, :, g], in1=gmax[:], op=ALU.subtract), ["lg", "gmax"], ["d4"])
            V("act", lambda e: e.activation(out=d4[:], in_=d4[:], func=AF.Exp), ["d4"], ["d4"])
            V("dve", lambda e: e.tensor_reduce(out=gs[:], in_=d4[:], axis=AX.X, op=ALU.add), ["d4"], ["gs"])
            V("dve", lambda e: e.reciprocal(out=gp[:], in_=gs[:]), ["gs"], ["gp"])
            V("dve", lambda e: e.tensor_scalar(out=pen[:], in0=og[:], scalar1=1e30, scalar2=-1e30, op0=ALU.mult, op1=ALU.add), ["og"], ["pen"])
            for ex in range(32):
                V("dve", lambda e, ex=ex: e.tensor_tensor(out=lm[:, :, ex], in0=lg[:, :, 4 + ex], in1=pen[:, :, ex // 8], op=ALU.add), ["lg", "pen"], ["lm"])
            V("dve", lambda e: e.tensor_reduce(out=m1[:], in_=lm[:], axis=AX.X, op=ALU.max), ["lm"], ["m1"])
            for ex in range(32):
                V("dve", lambda e, ex=ex: e.tensor_tensor(out=o1[:, :, ex], in0=lm[:, :, ex], in1=m1[:], op=ALU.is_equal), ["lm", "m1"], ["o1"])
            V("dve", lambda e: e.scalar_tensor_tensor(out=lm2[:], in0=o1[:], scalar=-1e30, in1=lm[:], op0=ALU.mult, op1=ALU.add), ["o1", "lm"], ["lm2"])
            V("dve", lambda e: e.tensor_reduce(out=m2[:], in_=lm2[:], axis=AX.X, op=ALU.max), ["lm2"], ["m2"])
            for ex in range(32):
                V("dve", lambda e, ex=ex: e.tensor_tensor(out=o2[:, :, ex], in0=lm2[:, :, ex], in1=m2[:], op=ALU.is_equal), ["lm2", "m2"], ["o2"])
            V("dve", lambda e: e.tensor_tensor(out=rr[:], in0=m2[:], in1=m1[:], op=ALU.subtract), ["m1", "m2"], ["rr"])
            V("act", lambda e: e.activation(out=rr[:], in_=rr[:], func=AF.Exp), ["rr"], ["rr"])
            V("dve", lambda e: e.tensor_scalar(out=den[:], in0=rr[:], scalar1=1.0, scalar2=None, op0=ALU.add), ["rr"], ["den"])
            V("dve", lambda e: e.reciprocal(out=den[:], in_=den[:]), ["den"], ["den"])
            V("dve", lambda e: e.tensor_tensor(out=gate[:, 0, :], in0=gp[:], in1=den[:], op=ALU.mult), ["gp", "den"], ["gate"])
            V("dve", lambda e: e.tensor_tensor(out=gate[:, 1, :], in0=gate[:, 0, :], in1=rr[:], op=ALU.mult), ["gate", "rr"], ["gate"])
            V("dve", lambda e: e.tensor_tensor(out=Mb[:], in0=o1[:], in1=o2[:], op=ALU.add), ["o1", "o2"], ["Mb"])
            for i in range(NT):
                pi = i % 2
                V("pe", lambda e, i=i, pi=pi: e.matmul(PS[pi][:, 0:32], lhsT=utb[:], rhs=Mb[:, i, :], start=True, stop=(i == 0)), ["utb", "Mb"], [PK[pi]])
                for jx in range(i):
                    V("pe", lambda e, i=i, jx=jx, pi=pi: e.matmul(PS[pi][:, 0:32], lhsT=onesb[:], rhs=Mb[:, jx, :], start=False, stop=(jx == i - 1)),
                      ["onesb", "Mb"], [PK[pi]])
                V("dve", lambda e, i=i, pi=pi: e.tensor_copy(out=rk[:, i, :], in_=PS[pi][:, 0:32]), [PK[pi]], ["rk"])
            for jx in range(NT):
                V("pe", lambda e, jx=jx: e.matmul(PS[2][:, 0:32], lhsT=onesb[:], rhs=Mb[:, jx, :], start=(jx == 0), stop=(jx == NT - 1)), ["onesb", "Mb"], [PK[2]])
            V("dve", lambda e: e.tensor_copy(out=cnt[:], in_=PS[2][:, 0:32]), [PK[2]], ["cnt"])
            V("dve", lambda e: e.tensor_single_scalar(out=nb_[:], in_=cnt[:], scalar=0.0, op=ALU.is_gt), ["cnt"], ["nb"])
            for jx in range(1, 10):
                V("dve", lambda e, jx=jx: e.tensor_single_scalar(out=tm[:], in_=cnt[:], scalar=float(jx * BLK), op=ALU.is_gt), ["cnt"], ["tm"])
                V("dve", lambda e: e.tensor_tensor(out=nb_[:], in0=nb_[:], in1=tm[:], op=ALU.add), ["nb", "tm"], ["nb"])
            V("dve", lambda e: e.tensor_copy(out=pe_[:, 0:1], in_=nb_[:, 0:1]), ["nb"], ["pe"])
            for ex in range(1, 32):
                V("dve", lambda e, ex=ex: e.tensor_tensor(out=pe_[:, ex:ex + 1], in0=pe_[:, ex - 1:ex], in1=nb_[:, ex:ex + 1], op=ALU.add), ["pe", "nb"], ["pe"])
            V("dve", lambda e: e.tensor_tensor(out=pst[:], in0=pe_[:], in1=nb_[:], op=ALU.subtract), ["pe", "nb"], ["pst"])
            V("dve", lambda e: e.tensor_scalar(out=pst[:], in0=pst[:], scalar1=float(BLK), scalar2=None, op0=ALU.mult), ["pst"], ["pst"])
            for i in range(NT):
                V("dve", lambda e, i=i: e.tensor_tensor(out=rk[:, i, :], in0=rk[:, i, :], in1=pst[:], op=ALU.add), ["rk", "pst"], ["rk"])
            for k, ok in enumerate([o1, o2]):
                V("dve", lambda e, ok=ok: e.tensor_tensor(out=pr[:], in0=ok[:], in1=rk[:], op=ALU.mult), ["o1", "o2", "rk"], ["pr"])
                V("dve", lambda e, k=k: e.tensor_reduce(out=slf[:, k, :], in_=pr[:], axis=AX.X, op=ALU.add), ["pr"], ["slf"])
            V("dve", lambda e: e.tensor_copy(out=slot_i[:], in_=slf[:]), ["slf"], ["slot_i"])
            V("dve", lambda e: e.memset(be[:], 0.0), [], ["be"])
            for ex in range(32):
                V("dve", lambda e, ex=ex: e.scalar_tensor_tensor(out=be[:], in0=cst2[:, 0:64], scalar=pe_[:, ex:ex + 1], in1=be[:], op0=ALU.is_ge, op1=ALU.add),
                  ["cst2", "pe", "be"], ["be"])
            V("dve", lambda e: e.tensor_scalar(out=be[:], in0=be[:], scalar1=128.0, scalar2=cst2[:, 64:65], op0=ALU.mult, op1=ALU.add), ["be", "cst2"], ["be"])
            V("dve", lambda e: e.tensor_copy(out=widx[:], in_=be[:, 0:NBLK]), ["be"], ["widx"])
            hs = [b.sb(S, "hs", [128, D], BF16) for _ in range(3)]
            for i in range(NT):
                H = hs[i % 3]
                kh_ = "hs%d" % (i % 3)
                b.dma("sp", lambda q, H=H, i=i: q.dma_start(out=H[:], in_=HTOK[i * 128:(i + 1) * 128, :]), reads=["HTOK"], writes=[kh_])
                for k in range(2):
                    b.dma("pool", lambda q, H=H, i=i, k=k: q.indirect_dma_start(out=XS, out_offset=bass.IndirectOffsetOnAxis(ap=slot_i[:, k, i:i + 1], axis=0),
                                                                              in_=H[:], in_offset=None), reads=[kh_, "slot_i"], writes=["XS"])
        b.barrier()

    BCREG = nc.gpsimd.to_reg(32 * 128 - 1)

    def phase_moe_blocks(li):
        with ExitStack() as S:
            xsb = b.sb(S, "xsb", [128, 2, D], BF16)
            xsT = [b.sb(S, "xsT", [128, KC, 256], BF16) for _ in range(2)]
            w13b = [[b.sb(S, "w13b", [128, 4096], BF16) for _ in range(4)] for _ in range(2)]
            w2b = [[b.sb(S, "w2b", [128, 2048], BF16) for _ in range(4)] for _ in range(2)]
            wst = [b.sb(S, "wst", [128, 4096], F32) for _ in range(2)]
            wst2 = [b.sb(S, "wst2", [128, 2048], F32) for _ in range(2)]
            sg = [b.sb(S, "sg", [128, 256], F32) for _ in range(2)]
            gT = [b.sb(S, "gT", [128, 4, 256], BF16) for _ in range(2)]
            yst = [b.sb(S, "yst", [128, D], BF16) for _ in range(2)]
            xsv = XS.rearrange("(j s p) d -> j p s d", s=2, p=128)
            cnt = {"w": 0, "w2": 0, "o": 0, "y": 0}

            def load13(j, m):
                r = j % 2
                wi = cnt["w"] % 2
                cnt["w"] += 1
                b.dma("pool", lambda q: q.indirect_dma_start(out=wst[wi][:], out_offset=None, in_=w13_in[li][m],
                                                             in_offset=bass.IndirectOffsetOnAxis(ap=widx[:, j:j + 1], axis=0), bounds_check=BCREG, oob_is_err=False),
                      reads=["widx"], writes=["wst%d" % wi])
                b.op("dve", lambda e: e.tensor_copy(out=w13b[r][m][:], in_=wst[wi][:]), reads=["wst%d" % wi], writes=["w13b%d%d" % (r, m)])

            def load2(j, m):
                r = j % 2
                wi = cnt["w2"] % 2
                cnt["w2"] += 1
                b.dma("pool", lambda q: q.indirect_dma_start(out=wst2[wi][:], out_offset=None, in_=w2_in[li][m],
                                                             in_offset=bass.IndirectOffsetOnAxis(ap=widx[:, j:j + 1], axis=0), bounds_check=BCREG, oob_is_err=False),
                      reads=["widx"], writes=["wst2%d" % wi])
                b.op("act", lambda e: e.activation(out=w2b[r][m][:], in_=wst2[wi][:], func=AF.Copy), reads=["wst2%d" % wi], writes=["w2b%d%d" % (r, m)])

            def loadxs(j):
                b.dma("sp", lambda q: q.dma_start(out=xsb[:], in_=xsv[j]), reads=["XS"], writes=["xsb"])

            def transposes(j):
                r = j % 2
                for s2 in range(2):
                    for hh in range(2):
                        pi = (s2 * 2 + hh) % 2
                        for cc in range(8):
                            c = hh * 8 + cc
                            b.op("pe", lambda e, s2=s2, c=c, cc=cc, pi=pi: e.transpose(PS[pi][:].bitcast(BF16)[:, cc * 128:(cc + 1) * 128],
                                                                                       xsb[:, s2, c * 128:(c + 1) * 128], identb[:]),
                                 reads=["xsb", "identb"], writes=[PK[pi]])
                        b.op("act", lambda e, r=r, s2=s2, hh=hh, pi=pi: e.activation(out=xsT[r][:, hh * 8:(hh + 1) * 8, s2 * 128:(s2 + 1) * 128],
                                                                                     in_=PS[pi][:].bitcast(BF16).rearrange("p (c t) -> p c t", c=8), func=AF.Copy),
                             reads=[PK[pi]], writes=["xsT%d" % r])

            loadxs(0)
            for m in range(4):
                load13(0, m)
                load2(0, m)
            transposes(0)
            loadxs(1)
            for j in range(NBLK):
                r = j % 2
                for m in range(4):
                    pa = 2 + 2 * (m % 2)
                    for half in range(2):
                        for c in range(KC):
                            b.op("pe", lambda e, r=r, m=m, half=half, c=c, pa=pa: e.matmul(PS[pa + half][:, 0:256],
                                                                                          lhsT=w13b[r][m][:, c * 256 + half * 128:c * 256 + half * 128 + 128],
                                                                                          rhs=xsT[r][:, c, :], start=(c == 0), stop=(c == KC - 1)),
                                 reads=["w13b%d%d" % (r, m), "xsT%d" % r], writes=[PK[pa + half]])
                    si = m % 2
                    b.op("act", lambda e, si=si, pa=pa: e.activation(out=sg[si][:], in_=PS[pa][:, 0:256], func=AF.Silu), reads=[PK[pa]], writes=["sg%d" % si])
                    b.op("dve", lambda e, si=si, pa=pa, r=r, m=m: e.tensor_tensor(out=gT[r][:, m, :], in0=PS[pa + 1][:, 0:256], in1=sg[si][:], op=ALU.mult),
                         reads=[PK[pa + 1], "sg%d" % si], writes=["gT%d" % r])
                    if j + 1 < NBLK:
                        load13(j + 1, m)
                        load2(j + 1, m)
                if j + 1 < NBLK:
                    transposes(j + 1)
                    if j + 2 < NBLK:
                        loadxs(j + 2)
                for s2 in range(2):
                    Y = yst[cnt["y"] % 2]
                    ky = "yst%d" % (cnt["y"] % 2)
                    cnt["y"] += 1
                    for nbk in range(4):
                        po = 6 + cnt["o"] % 2
                        cnt["o"] += 1
                        for c4 in range(4):
                            b.op("pe", lambda e, r=r, s2=s2, nbk=nbk, c4=c4, po=po: e.matmul(PS[po][:], lhsT=gT[r][:, c4, s2 * 128:(s2 + 1) * 128],
                                                                                            rhs=w2b[r][nbk][:, c4 * 512:(c4 + 1) * 512], start=(c4 == 0), stop=(c4 == 3)),
                                 reads=["gT%d" % r, "w2b%d%d" % (r, nbk)], writes=[PK[po]])
                        b.op("act", lambda e, Y=Y, nbk=nbk, po=po: e.activation(out=Y[:, nbk * 512:(nbk + 1) * 512], in_=PS[po][:], func=AF.Copy), reads=[PK[po]], writes=[ky])
                    b.dma("sp", lambda q, Y=Y, j=j, s2=s2: q.dma_start(out=YS[j * 256 + s2 * 128:j * 256 + (s2 + 1) * 128, :], in_=Y[:]), reads=[ky], writes=["YS"])
        b.barrier()

    def moe_ysrc(gidx):
        def mk(S):
            yg = [[b.sb(S, "yg", [128, D], BF16) for _ in range(2)] for _ in range(2)]
            ff = [b.sb(S, "ff", [128, D], F32) for _ in range(2)]
            st = {"n": 0}

            def zfn(tbi, t0, j, c, X, kx):
                if c == 0:
                    for tt in range(2):
                        ti = tbi * 2 + tt
                        for k in range(2):
                            b.dma("pool", lambda q, tt=tt, k=k, ti=ti: q.indirect_dma_start(out=yg[tt][k][:], out_offset=None, in_=YS,
                                                                                          in_offset=bass.IndirectOffsetOnAxis(ap=slot_i[:, k, ti:ti + 1], axis=0)),
                                  reads=["YS", "slot_i"], writes=["yg%d%d" % (tt, k)])
                        b.op("act", lambda e, tt=tt, ti=ti: e.activation(out=ff[tt][:], in_=yg[tt][0][:], func=AF.Copy, scale=gate[:, 0, ti:ti + 1]),
                             reads=["yg%d0" % tt, "gate"], writes=["ff%d" % tt])
                        b.op("dve", lambda e, tt=tt, ti=ti: e.scalar_tensor_tensor(out=ff[tt][:], in0=yg[tt][1][:], scalar=gate[:, 1, ti:ti + 1], in1=ff[tt][:],
                                                                                  op0=ALU.mult, op1=ALU.add), reads=["yg%d1" % tt, "gate", "ff%d" % tt], writes=["ff%d" % tt])
                pi = st["n"] % 2
                st["n"] += 1
                for tt in range(2):
                    b.op("pe", lambda e, tt=tt: e.transpose(PS[pi][:, tt * 128:(tt + 1) * 128], ff[tt][:, c * 128:(c + 1) * 128], identf),
                         reads=["ff%d" % tt, "cst"], writes=[PK[pi]])
                b.op("dve", lambda e: e.scalar_tensor_tensor(out=X[:, c, :], in0=PS[pi][:, 0:256], scalar=der[:, gidx, c, j:j + 1], in1=X[:, c, :],
                                                              op0=ALU.mult, op1=ALU.add), reads=[PK[pi], kx, "der"], writes=[kx])
            return zfn
        return mk


    esink = b.sb(G, "esink", [128, 16], F32)

    def swa_qblocks(h):
        res = []
        ctxc = [(2048, "none", None), (2176, "none", None)]
        for bq in range(4):
            q0 = bq * 512
            ch = []
            for jx in range(6):
                k0 = q0 - 128 + jx * 128
                if 0 <= k0 < L:
                    ch.append((k0, "mask", jx))
            res.append((q0, 512, ch + ctxc))
        res.append((2048, 256, ctxc))
        return res

    def phase_attn_swa():
        b.op("act", lambda e: e.activation(out=esink[:], in_=vcol(2, "sink", 0, 16), func=AF.Exp), reads=["vec2"], writes=["esink"])
        with ExitStack() as S:
            heads = [(h * 128, (h // 4) * 128, (h // 4) * 128, h) for h in range(16)]
            attn_head_loop(S, heads, 128, swa_qblocks, simple_finish(S), extra_den=lambda h: esink[:, h:h + 1])
        b.barrier()

    LAM_INIT = 0.8 - 0.6 * math.exp(-0.3 * 3)
    nlam = b.sb(G, "nlam", [128, 1], F32)
    subgl = b.sb(G, "subgl", [128, 2], F32)
    lamt = b.sb(G, "lamt", [128, 2], F32)

    def diff_qblocks(tag):
        allk = [(i * 128, "none", None) for i in range(NT)]
        res = [(bq * 512, 512, allk) for bq in range(4)]
        res.append((2048, 256, [(2048, "none", None), (2176, "none", None)]))
        return res

    def phase_attn_diff():
        R3 = ["vec3"]
        b.op("dve", lambda e: e.tensor_tensor(out=lamt[:, 0:1], in0=vcol(3, "lam", 0), in1=vcol(3, "lam", 1), op=ALU.mult), reads=R3, writes=["lamt"])
        b.op("dve", lambda e: e.tensor_tensor(out=lamt[:, 1:2], in0=vcol(3, "lam", 2), in1=vcol(3, "lam", 3), op=ALU.mult), reads=R3, writes=["lamt"])
        b.op("pe", lambda e: e.matmul(PS[5][:, 0:2], lhsT=cst[:, 1, :], rhs=lamt[:], start=True, stop=True), reads=["cst", "lamt"], writes=[PK[5]])
        b.op("act", lambda e: e.activation(out=lamt[:], in_=PS[5][:, 0:2], func=AF.Exp), reads=[PK[5]], writes=["lamt"])
        b.op("dve", lambda e: e.tensor_tensor(out=nlam[:], in0=lamt[:, 1:2], in1=lamt[:, 0:1], op=ALU.subtract), reads=["lamt"], writes=["nlam"])
        b.op("dve", lambda e: e.tensor_scalar(out=nlam[:], in0=nlam[:], scalar1=-LAM_INIT, scalar2=None, op0=ALU.add), reads=["nlam"], writes=["nlam"])
        b.op("dve", lambda e: e.tensor_scalar(out=subgl[:], in0=vcol(3, "subg", 0, 2), scalar1=1.0 - LAM_INIT, scalar2=None, op0=ALU.mult), reads=R3, writes=["subgl"])
        with ExitStack() as S:
            o0 = b.sb(S, "o0", [128, 2, T], F32)
            od = [b.sb(S, "od", [128, 2, 512], F32) for _ in range(2)]
            osq = [b.sb(S, "osq", [128, 512], BF16) for _ in range(2)]
            rs = [b.sb(S, "rs", [128, 512], F32) for _ in range(2)]
            ostg = [b.sb(S, "dostg", [128, 2, T], BF16) for _ in range(2)]
            st = {"n": 0}

            def fin(hn, tag, q0, nq, rec, krec, ob, rb):
                h, m = tag
                if m == 0:
                    for dv in range(2):
                        b.op("dve", lambda e, dv=dv: e.tensor_tensor(out=o0[:, dv, q0:q0 + nq], in0=PS[ob[dv]][:, 0:nq], in1=rec[:, 0:nq], op=ALU.mult),
                             reads=[PK[ob[dv]], krec], writes=["o0"])
                    return
                i = st["n"] % 2
                st["n"] += 1
                O = od[i]
                ko = "od%d" % i
                og = ostg[h % 2]
                kg = "dostg%d" % (h % 2)
                for dv in range(2):
                    b.op("dve", lambda e, dv=dv: e.tensor_tensor(out=O[:, dv, 0:nq], in0=PS[ob[dv]][:, 0:nq], in1=rec[:, 0:nq], op=ALU.mult),
                         reads=[PK[ob[dv]], krec], writes=[ko])
                    b.op("dve", lambda e, dv=dv: e.scalar_tensor_tensor(out=O[:, dv, 0:nq], in0=O[:, dv, 0:nq], scalar=nlam[:, 0:1], in1=o0[:, dv, q0:q0 + nq],
                                                                        op0=ALU.mult, op1=ALU.add), reads=[ko, "nlam", "o0"], writes=[ko])
                    b.op("act", lambda e, dv=dv: e.activation(out=osq[dv][:, 0:nq], in_=O[:, dv, 0:nq], func=AF.Square), reads=[ko], writes=["osq%d" % dv])
                    b.op("pe", lambda e, dv=dv: e.matmul(PS[rb][:, 0:nq], lhsT=onesb[:], rhs=osq[dv][:, 0:nq], start=(dv == 0), stop=(dv == 1)),
                         reads=["osq%d" % dv, "onesb"], writes=[PK[rb]])
                R_ = rs[i]
                kr = "rs%d" % i
                b.op("dve", lambda e: e.tensor_scalar(out=R_[:, 0:nq], in0=PS[rb][:, 0:nq], scalar1=1.0 / 256.0, scalar2=EPS, op0=ALU.mult, op1=ALU.add),
                     reads=[PK[rb]], writes=[kr])
                b.op("act", lambda e: e.activation(out=R_[:, 0:nq], in_=R_[:, 0:nq], func=AF.Sqrt), reads=[kr], writes=[kr])
                b.op("dve", lambda e: e.reciprocal(out=R_[:, 0:nq], in_=R_[:, 0:nq]), reads=[kr], writes=[kr])
                for dv in range(2):
                    b.op("dve", lambda e, dv=dv: e.tensor_tensor(out=O[:, dv, 0:nq], in0=O[:, dv, 0:nq], in1=R_[:, 0:nq], op=ALU.mult), reads=[ko, kr], writes=[ko])
                    b.op("act", lambda e, dv=dv: e.activation(out=og[:, dv, q0:q0 + nq], in_=O[:, dv, 0:nq], func=AF.Copy, scale=subgl[:, dv:dv + 1]),
                         reads=[ko, "subgl"], writes=[kg])
                if q0 + nq == T:
                    for dv in range(2):
                        b.dma("sp", lambda q, dv=dv: q.dma_start(out=OT[h * 256 + dv * 128:h * 256 + (dv + 1) * 128, :], in_=og[:, dv, :]),
                              reads=[kg], writes=[("dstT", id(OT))])

            heads = [((2 * h + m) * 128, (2 * h + m) * 128, h * 256, (h, m)) for h in range(8) for m in range(2)]
            attn_head_loop(S, heads, 256, diff_qblocks, fin)
        b.barrier()

    GL = 15 + L + 15
    GW = GL + 15 + C + 15

    def phase_conv_a(XT):
        with ExitStack() as S:
            uT = build_u(S, XT)
            wq = [b.sb(S, "wgl", [128, KC, 512], BF16) for _ in range(2)]
            Gb = [b.sb(S, "Gb", [128, GW], F32) for _ in range(2)]
            acc = [b.sb(S, "cacc", [128, T], F32) for _ in range(2)]
            sig = [b.sb(S, "sig", [128, 512], F32) for _ in range(2)]
            for i in range(2):
                b.op("pool", lambda e, i=i: e.memset(Gb[i][:], 0.0), writes=["Gb%d" % i])
            src = win1.rearrange("(c p) n -> p c n", p=128)
            cnt = 0
            for pc in range(8):
                wb = wq[pc % 2]
                kw = "wgl%d" % (pc % 2)
                b.dma("pool", lambda q, wb=wb, pc=pc: q.dma_start(out=wb[:], in_=src[:, :, pc * 512:(pc + 1) * 512]), writes=[kw])
                for f2 in range(2):
                    fc = pc * 2 + f2
                    Gt = Gb[fc % 2]
                    kg = "Gb%d" % (fc % 2)
                    A = acc[fc % 2]
                    ka = "cacc%d" % (fc % 2)
                    for (t0, tn, j) in TB512:
                        pi = cnt % 2
                        cnt += 1
                        for half in range(2):
                            for k in range(KC):
                                b.op("pe", lambda e, k=k, half=half, pi=pi, t0=t0, tn=tn, wb=wb, f2=f2: e.matmul(
                                    PS[2 * pi + half][:, 0:tn], lhsT=wb[:, k, f2 * 256 + half * 128:f2 * 256 + half * 128 + 128],
                                    rhs=uT[:, k, t0:t0 + tn], start=(k == 0), stop=(k == KC - 1)), reads=[kw, "uT"], writes=[PK[2 * pi + half]])
                        b.op("act", lambda e, pi=pi, tn=tn, fc=fc: e.activation(out=sig[pi][:, 0:tn], in_=PS[2 * pi + 1][:, 0:tn], func=AF.Sigmoid,
                                                                               bias=vcol(1, "b_in", 16 + fc)), reads=[PK[2 * pi + 1], "vec1"], writes=["sig%d" % pi])
                        g0 = 15 + t0 if j == 0 else GL + 15
                        b.op("dve", lambda e, pi=pi, tn=tn, fc=fc, g0=g0, Gt=Gt: e.scalar_tensor_tensor(
                            out=Gt[:, g0:g0 + tn], in0=PS[2 * pi][:, 0:tn], scalar=vcol(1, "b_in", fc), in1=sig[pi][:, 0:tn], op0=ALU.add, op1=ALU.mult),
                            reads=[PK[2 * pi], "sig%d" % pi, "vec1"], writes=[kg])
                    for (a0, an, gb) in [(0, L, 0), (L, C, GL)]:
                        b.op("dve", lambda e, A=A, Gt=Gt, a0=a0, an=an, gb=gb, fc=fc: e.tensor_scalar(
                            out=A[:, a0:a0 + an], in0=Gt[:, gb:gb + an], scalar1=vcol(1, "dw", fc), scalar2=vcol(1, "dw_b", fc), op0=ALU.mult, op1=ALU.add),
                            reads=[kg, "vec1"], writes=[ka])
                        for k in range(1, 31):
                            b.op("dve", lambda e, A=A, Gt=Gt, a0=a0, an=an, gb=gb, fc=fc, k=k: e.scalar_tensor_tensor(
                                out=A[:, a0:a0 + an], in0=Gt[:, gb + k:gb + k + an], scalar=vcol(1, "dw", k * 16 + fc), in1=A[:, a0:a0 + an],
                                op0=ALU.mult, op1=ALU.add), reads=[kg, ka, "vec1"], writes=[ka])
                    b.dma("sp", lambda q, A=A, fc=fc: q.dma_start(out=CT[fc * 128:(fc + 1) * 128, :], in_=A[:]), reads=[ka], writes=["CT"])
        b.barrier()

    def phase_conv_b():
        with ExitStack() as S:
            xz = [b.sb(S, "cz", [128, KC, 256], F32) for _ in range(2)]
            ob = [b.sb(S, "cob", [128, KC, 256], BF16) for _ in range(2)]
            zb = [b.sb(S, "czb", [128, 256], BF16) for _ in range(3)]
            zq = [b.sb(S, "czq", [128, 256], BF16) for _ in range(3)]
            mean = b.sb(S, "cmean", [128, 256], F32)
            rstd = b.sb(S, "crstd", [128, 256], F32)
            var = b.sb(S, "cvar", [128, 256], F32)
            src = CT.rearrange("(c p) t -> p c t", p=128)
            dst = OT.rearrange("(c p) t -> p c t", p=128)
            cz = 0
            for tbi, (t0, tn, j) in enumerate(TB256):
                X = xz[tbi % 2]
                kx = "cz%d" % (tbi % 2)
                O = ob[tbi % 2]
                ko = "cob%d" % (tbi % 2)
                b.dma("sp", lambda q, X=X, t0=t0: q.dma_start(out=X[:], in_=src[:, :, t0:t0 + 256]), reads=["CT"], writes=[kx])
                for c in range(KC):
                    zi = cz % 3
                    cz += 1
                    b.op("act", lambda e, zi=zi, X=X, c=c: e.activation(out=zb[zi][:], in_=X[:, c, :], func=AF.Copy), reads=[kx], writes=["czb%d" % zi])
                    b.op("act", lambda e, zi=zi, X=X, c=c: e.activation(out=zq[zi][:], in_=X[:, c, :], func=AF.Square), reads=[kx], writes=["czq%d" % zi])
                    b.op("pe", lambda e, zi=zi, c=c: e.matmul(PS[2][:, 0:256], lhsT=onesb[:], rhs=zb[zi][:], start=(c == 0), stop=(c == KC - 1)),
                         reads=["czb%d" % zi, "onesb"], writes=[PK[2]])
                    b.op("pe", lambda e, zi=zi, c=c: e.matmul(PS[3][:, 0:256], lhsT=onesb[:], rhs=zq[zi][:], start=(c == 0), stop=(c == KC - 1)),
                         reads=["czq%d" % zi, "onesb"], writes=[PK[3]])
                b.op("dve", lambda e: e.tensor_scalar(out=mean[:], in0=PS[2][:, 0:256], scalar1=1.0 / D, scalar2=None, op0=ALU.mult), reads=[PK[2]], writes=["cmean"])
                b.op("dve", lambda e: e.tensor_tensor(out=var[:], in0=mean[:], in1=mean[:], op=ALU.mult), reads=["cmean"], writes=["cvar"])
                b.op("dve", lambda e: e.scalar_tensor_tensor(out=var[:], in0=PS[3][:, 0:256], scalar=1.0 / D, in1=var[:], op0=ALU.mult, op1=ALU.subtract),
                     reads=[PK[3], "cvar"], writes=["cvar"])
                b.op("dve", lambda e: e.tensor_scalar(out=var[:], in0=var[:], scalar1=EPS, scalar2=None, op0=ALU.add), reads=["cvar"], writes=["cvar"])
                b.op("act", lambda e: e.activation(out=var[:], in_=var[:], func=AF.Sqrt), reads=["cvar"], writes=["cvar"])
                b.op("dve", lambda e: e.reciprocal(out=rstd[:], in_=var[:]), reads=["cvar"], writes=["crstd"])
                for c in range(KC):
                    b.op("dve", lambda e, X=X, c=c: e.tensor_tensor(out=X[:, c, :], in0=X[:, c, :], in1=mean[:], op=ALU.subtract), reads=[kx, "cmean"], writes=[kx])
                    b.op("dve", lambda e, X=X, c=c: e.tensor_tensor(out=X[:, c, :], in0=X[:, c, :], in1=rstd[:], op=ALU.mult), reads=[kx, "crstd"], writes=[kx])
                    b.op("act", lambda e, X=X, O=O, c=c: e.activation(out=X[:, c, :], in_=X[:, c, :], func=AF.Identity,
                                                                      scale=vcol(1, "cln_g", c), bias=vcol(1, "cln_b", c)), reads=[kx, "vec1"], writes=[kx])
                    b.op("act", lambda e, X=X, O=O, c=c: e.activation(out=O[:, c, :], in_=X[:, c, :], func=AF.Silu), reads=[kx], writes=[ko])
                b.dma("sp", lambda q, O=O, t0=t0: q.dma_start(out=dst[:, :, t0:t0 + 256], in_=O[:]), reads=[ko], writes=[("dstT", id(OT))])
        b.barrier()

    phase_mod()
    stop = {"l0": 0, "l1": 1, "l2": 2, "l3": 3}.get(dbg, 3)
    Xin = xT_in
    for li in range(stop + 1):
        derive(li)
        if li == 0:
            phase_premix_attn(0, Xin, wqkv0, 16, 16, 2048, False)
            phase_attn_na()
            ln_phase(0, Xin, XA, wo_ysrc(wo0, 2), 2, "ln1_g", "ln1_b", True)
        elif li == 1:
            phase_conv_a(Xin)
            phase_conv_b()
            ln_phase(1, Xin, XA, wo_ysrc(wo1, 2, "b_out", 1), 2, "ln1_g", "ln1_b", True)
        elif li == 2:
            phase_premix_attn(2, Xin, wqkv2, 16, 4, 512, True)
            phase_attn_swa()
            ln_phase(2, Xin, XA, wo_ysrc(wo2, 2), 2, "ln1_g", "ln1_b", True)
        else:
            phase_premix_attn(3, Xin, wqkv3, 16, 16, 2048, True)
            phase_attn_diff()
            ln_phase(3, Xin, XA, wo_ysrc(wo3, 2), 2, "ln1_g", "ln1_b", True)
        phase_moe_route(li)
        phase_moe_blocks(li)
        ln_phase(li, XA, XB, moe_ysrc(5), 5, "ln2_g", "ln2_b", False, final=(li == 3))
        Xin = XB
    finish(b)
    return b


def finish(b):
    b.barrier()


def host_consts():
    ident = np.eye(128, dtype=np.float32)
    ones = np.ones((128, 128), np.float32)
    P = np.zeros((128, 128), np.float32)
    for d in range(128):
        blk, r = divmod(d, 64)
        partner = blk * 64 + (r + 32) % 64
        P[partner, d] = 1.0
    UT = np.triu(np.ones((128, 128), np.float32), 1)
    cst = np.ascontiguousarray(np.stack([ident, ones, P, UT], 1))
    t = np.arange(L)
    pos = np.stack([t // 64, t % 64], -1).astype(np.float32)
    inv = (10000.0 ** (-np.arange(32, dtype=np.float32) / 32)).astype(np.float32)
    ang = pos[:, :, None] * inv
    cos = np.cos(ang).astype(np.float32)
    sin = np.sin(ang).astype(np.float32)
    Ct = np.ones((128, T), np.float32)
    St = np.zeros((128, T), np.float32)
    for ax in range(2):
        Ct[ax * 64:ax * 64 + 32, :L] = cos[:, ax, :].T
        Ct[ax * 64 + 32:ax * 64 + 64, :L] = cos[:, ax, :].T
        St[ax * 64:ax * 64 + 32, :L] = -sin[:, ax, :].T
        St[ax * 64 + 32:ax * 64 + 64, :L] = sin[:, ax, :].T
    rope = np.ascontiguousarray(np.stack([Ct, St], 0))
    swm = np.zeros((6, 128, 512), np.float32)
    for j in range(6):
        kp = (j - 1) * 128 + np.arange(128)[:, None]
        qp = np.arange(512)[None, :]
        swm[j] = (np.abs(qp - kp) <= 128).astype(np.float32)
    cst2 = np.zeros((128, 66), np.float32)
    cst2[:, 0:64] = np.arange(64, dtype=np.float32)[None, :]
    cst2[:, 64] = np.arange(128, dtype=np.float32)
    return cst, rope, swm, cst2


def na_bias_tiles(rpb):
    rows = 32
    out = np.full((16, 20, 128, 512), NEG, np.float32)
    combos = [(0, kc, kc) for kc in range(6)] + [(1, 2 + j, 6 + j) for j in range(8)] + [(3, kc, 14 + kc - 10) for kc in range(10, 16)]
    qi = np.arange(512)
    qr_l, qc = qi // 64, qi % 64
    ki = np.arange(128)
    kr_l, kcn = ki // 64, ki % 64
    for (bq, kc, ti) in combos:
        qr = bq * 8 + qr_l
        kr = kc * 2 + kr_l
        r0 = np.clip(qr - 4, 0, rows - 8)
        okr = (kr[:, None] >= r0[None, :]) & (kr[:, None] < r0[None, :] + 8)
        cs = np.clip(qc - 8, 0, 64 - 16)
        okc = (kcn[:, None] >= cs[None, :]) & (kcn[:, None] < cs[None, :] + 16)
        dr = np.clip(kr[:, None] - qr[None, :] + 7, 0, 14)
        dc = np.clip(kcn[:, None] - qc[None, :] + 15, 0, 30)
        g = rpb[:, dr, dc]
        out[:, ti] = np.where((okr & okc)[None], g, np.float32(NEG))
    return out


def host_prepare(inp):
    cst, rope, swm, cst2 = host_consts()
    shared = {"cst": cst, "rope": rope, "swm": swm, "cst2": cst2}
    shared["nab"] = na_bias_tiles(np.asarray(inp["l0_na_rpb"], np.float32))
    kinds = [0, 1, 2, 3]
    for i in range(4):
        p = "l%d_" % i
        cols, n = vec_layout(kinds[i])
        v = np.zeros((128, n), np.float32)
        v[:, cols["mod_b"]:cols["mod_b"] + 96] = pk(inp[p + "mod_b"])
        for nm in ["ln1_g", "ln1_b", "ln2_g", "ln2_b"]:
            v[:, cols[nm]:cols[nm] + 16] = pk(inp[p + nm])
        rb = np.concatenate([inp[p + "router_g_b"], inp[p + "router_e_b"]]).astype(np.float32)
        v[:, cols["rb"]:cols["rb"] + 36] = rb[None, :]
        if i == 1:
            v[:, cols["b_in"]:cols["b_in"] + 32] = pk(inp[p + "cv_b_in"])
            dw = np.asarray(inp[p + "cv_dw"], np.float32)
            for k in range(31):
                v[:, cols["dw"] + k * 16:cols["dw"] + (k + 1) * 16] = pk(dw[k])
            v[:, cols["dw_b"]:cols["dw_b"] + 16] = pk(inp[p + "cv_dw_b"])
            v[:, cols["cln_g"]:cols["cln_g"] + 16] = pk(inp[p + "cv_ln_g"])
            v[:, cols["cln_b"]:cols["cln_b"] + 16] = pk(inp[p + "cv_ln_b"])
            v[:, cols["b_out"]:cols["b_out"] + 16] = pk(inp[p + "cv_b_out"])
        if i == 2:
            v[:, cols["sink"]:cols["sink"] + 16] = np.asarray(inp[p + "sw_sink"], np.float32)[None, :]
        if i == 3:
            v[:, cols["subg"]:cols["subg"] + 2] = pk(inp[p + "df_subln_g"])
            v[:, cols["lam"]:cols["lam"] + 4] = np.asarray(inp[p + "df_lambda"], np.float32).T
        shared["vec%d" % i] = v
        shared["modw%d" % i] = np.asarray(inp[p + "mod_w"], np.float32)
        rwm = np.concatenate([inp[p + "router_g_w"], inp[p + "router_e_w"]], 1).astype(np.float32)
        shared["rw%d" % i] = np.ascontiguousarray(rwm.reshape(KC, 128, 36).transpose(1, 0, 2))
        w13 = np.asarray(inp[p + "moe_w13"], np.float32)
        w = w13.reshape(32, KC, 128, 2, 4, 128)
        w = w.transpose(0, 2, 4, 1, 3, 5)
        for m in range(4):
            shared["w13_%d_%d" % (i, m)] = np.ascontiguousarray(w[:, :, m]).reshape(32 * 128, 4096)
        w2 = np.asarray(inp[p + "moe_w2"], np.float32)
        w = w2.reshape(32, 4, 128, 4, 512).transpose(0, 2, 3, 1, 4)
        for m in range(4):
            shared["w2_%d_%d" % (i, m)] = np.ascontiguousarray(w[:, :, m]).reshape(32 * 128, 2048)
    shared["wqkv0"] = np.asarray(inp["l0_na_w_qkv"], np.float32)
    shared["wo0"] = np.asarray(inp["l0_na_w_o"], np.float32)
    win = np.asarray(inp["l1_cv_w_in"], np.float32)
    shared["win1"] = np.ascontiguousarray(np.stack([win[:, :D].reshape(D, KC, 128), win[:, D:].reshape(D, KC, 128)], 2)).reshape(D, 4096)
    shared["wo1"] = np.asarray(inp["l1_cv_w_out"], np.float32)
    shared["wqkv2"] = np.asarray(inp["l2_sw_w_qkv"], np.float32)
    shared["wo2"] = np.asarray(inp["l2_sw_w_o"], np.float32)
    shared["wqkv3"] = np.asarray(inp["l3_df_w_qkv"], np.float32)
    shared["wo3"] = np.asarray(inp["l3_df_w_o"], np.float32)
    x = np.asarray(inp["x"], np.float32)
    ctx = np.asarray(inp["ctx"], np.float32)
    c = np.asarray(inp["c"], np.float32)
    cc = np.asarray(inp["c_ctx"], np.float32)
    maps = []
    for bi in range(8):
        m = dict(shared)
        m["xT"] = np.ascontiguousarray(np.concatenate([x[bi], ctx[bi]], 0).T)
        m["cT"] = np.ascontiguousarray(np.stack([pk(c[bi]), pk(cc)], -1))
        maps.append(m)
    return maps


_CACHE = {}


def kernel(x, c, ctx, c_ctx, l0_mod_w, l0_mod_b, l0_na_w_qkv, l0_na_rpb, l0_na_w_o, l0_ln1_g, l0_ln1_b, l0_router_g_w, l0_router_g_b, l0_router_e_w, l0_router_e_b, l0_moe_w13, l0_moe_w2, l0_ln2_g, l0_ln2_b, l1_mod_w, l1_mod_b, l1_cv_w_in, l1_cv_b_in, l1_cv_dw, l1_cv_dw_b, l1_cv_ln_g, l1_cv_ln_b, l1_cv_w_out, l1_cv_b_out, l1_ln1_g, l1_ln1_b, l1_router_g_w, l1_router_g_b, l1_router_e_w, l1_router_e_b, l1_moe_w13, l1_moe_w2, l1_ln2_g, l1_ln2_b, l2_mod_w, l2_mod_b, l2_sw_w_qkv, l2_sw_sink, l2_sw_w_o, l2_ln1_g, l2_ln1_b, l2_router_g_w, l2_router_g_b, l2_router_e_w, l2_router_e_b, l2_moe_w13, l2_moe_w2, l2_ln2_g, l2_ln2_b, l3_mod_w, l3_mod_b, l3_df_w_qkv, l3_df_lambda, l3_df_subln_g, l3_df_w_o, l3_ln1_g, l3_ln1_b, l3_router_g_w, l3_router_g_b, l3_router_e_w, l3_router_e_b, l3_moe_w13, l3_moe_w2, l3_ln2_g, l3_ln2_b):
    inputs = dict(x=x, c=c, ctx=ctx, c_ctx=c_ctx, l0_mod_w=l0_mod_w, l0_mod_b=l0_mod_b, l0_na_w_qkv=l0_na_w_qkv, l0_na_rpb=l0_na_rpb, l0_na_w_o=l0_na_w_o, l0_ln1_g=l0_ln1_g, l0_ln1_b=l0_ln1_b, l0_router_g_w=l0_router_g_w, l0_router_g_b=l0_router_g_b, l0_router_e_w=l0_router_e_w, l0_router_e_b=l0_router_e_b, l0_moe_w13=l0_moe_w13, l0_moe_w2=l0_moe_w2, l0_ln2_g=l0_ln2_g, l0_ln2_b=l0_ln2_b, l1_mod_w=l1_mod_w, l1_mod_b=l1_mod_b, l1_cv_w_in=l1_cv_w_in, l1_cv_b_in=l1_cv_b_in, l1_cv_dw=l1_cv_dw, l1_cv_dw_b=l1_cv_dw_b, l1_cv_ln_g=l1_cv_ln_g, l1_cv_ln_b=l1_cv_ln_b, l1_cv_w_out=l1_cv_w_out, l1_cv_b_out=l1_cv_b_out, l1_ln1_g=l1_ln1_g, l1_ln1_b=l1_ln1_b, l1_router_g_w=l1_router_g_w, l1_router_g_b=l1_router_g_b, l1_router_e_w=l1_router_e_w, l1_router_e_b=l1_router_e_b, l1_moe_w13=l1_moe_w13, l1_moe_w2=l1_moe_w2, l1_ln2_g=l1_ln2_g, l1_ln2_b=l1_ln2_b, l2_mod_w=l2_mod_w, l2_mod_b=l2_mod_b, l2_sw_w_qkv=l2_sw_w_qkv, l2_sw_sink=l2_sw_sink, l2_sw_w_o=l2_sw_w_o, l2_ln1_g=l2_ln1_g, l2_ln1_b=l2_ln1_b, l2_router_g_w=l2_router_g_w, l2_router_g_b=l2_router_g_b, l2_router_e_w=l2_router_e_w, l2_router_e_b=l2_router_e_b, l2_moe_w13=l2_moe_w13, l2_moe_w2=l2_moe_w2, l2_ln2_g=l2_ln2_g, l2_ln2_b=l2_ln2_b, l3_mod_w=l3_mod_w, l3_mod_b=l3_mod_b, l3_df_w_qkv=l3_df_w_qkv, l3_df_lambda=l3_df_lambda, l3_df_subln_g=l3_df_subln_g, l3_df_w_o=l3_df_w_o, l3_ln1_g=l3_ln1_g, l3_ln1_b=l3_ln1_b, l3_router_g_w=l3_router_g_w, l3_router_g_b=l3_router_g_b, l3_router_e_w=l3_router_e_w, l3_router_e_b=l3_router_e_b, l3_moe_w13=l3_moe_w13, l3_moe_w2=l3_moe_w2, l3_ln2_g=l3_ln2_g, l3_ln2_b=l3_ln2_b)
    maps = host_prepare(inputs)
    if "nc" not in _CACHE:
        _CACHE["nc"] = build().nc
    res = run_bass_kernel_spmd(_CACHE["nc"], maps, core_ids=list(range(8)))
    out = np.stack([np.ascontiguousarray(r["outT"].T) for r in res.results], 0)
    return out.astype(np.float32)
```
